# Optimizing a Trainium2 kernel written in Bass

```python
import math
import jax, jax.numpy as jnp
from jax import lax
import numpy as np

D_MODEL = 1024
BATCH = 4
SEQ = 4096
DEPTH = 2

MLA_HEADS = 8
MLA_NOPE = 64
MLA_ROPE = 32
MLA_V = 64
MLA_Q_RANK = 256
MLA_KV_RANK = 128
ROPE_BASE = 10000.0
NSA_HEADS = 8
NSA_KV_HEADS = 2
NSA_DIM = 64
CMP_LEN = 32
CMP_STRIDE = 16
CMP_HIDDEN = 128
SLC_LEN = 64
SLC_TOPK = 16
WINDOW = 512
REL_BUCKETS = 32
REL_MAX_DIST = 128
D_FF = 2816
CONV_WIDTH = 3
PLE_DIM = 256
Q_BLOCK = 128
EPS = 1e-6
NEG = -1e30
POS_BIG = 1e30

IN_SPLITS = (MLA_Q_RANK, MLA_KV_RANK, MLA_ROPE, NSA_HEADS * NSA_DIM,
             NSA_KV_HEADS * NSA_DIM, NSA_KV_HEADS * NSA_DIM,
             NSA_KV_HEADS * NSA_DIM, NSA_KV_HEADS * NSA_DIM,
             NSA_KV_HEADS * NSA_DIM, NSA_KV_HEADS * NSA_DIM,
             3 * NSA_HEADS)
MIX_OUT = MLA_HEADS * MLA_V + NSA_HEADS * NSA_DIM

kernel_name = "hybrid_mla_nsa_convffn_trunk"


def rms_norm(x, g):
    xf = x.astype(jnp.float32)
    y = xf * lax.rsqrt(jnp.mean(xf * xf, axis=-1, keepdims=True) + EPS)
    return (y * g.astype(jnp.float32)).astype(x.dtype)


def rotary(x, cos, sin):
    half = x.shape[-1] // 2
    x1, x2 = x[..., :half], x[..., half:]
    return jnp.concatenate([x1 * cos - x2 * sin, x2 * cos + x1 * sin], axis=-1).astype(x.dtype)


def rel_bucket(dist):
    n = jnp.maximum(dist, 0)
    max_exact = REL_BUCKETS // 2
    large = max_exact + (jnp.log(jnp.maximum(n, 1).astype(jnp.float32) / max_exact)
                         / math.log(REL_MAX_DIST / max_exact)
                         * (REL_BUCKETS - max_exact)).astype(jnp.int32)
    large = jnp.minimum(large, REL_BUCKETS - 1)
    return jnp.where(n < max_exact, n, large)


def mla_attention(c_q, c_kv, k_rope, positions, q_norm, w_uq, kv_norm, w_ukv):
    B, S, _ = c_q.shape
    f32 = jnp.float32
    q = (rms_norm(c_q, q_norm) @ w_uq).reshape(B, S, MLA_HEADS, MLA_NOPE + MLA_ROPE)
    kv = (rms_norm(c_kv, kv_norm) @ w_ukv).reshape(B, S, MLA_HEADS, MLA_NOPE + MLA_V)
    half = MLA_ROPE // 2
    inv = ROPE_BASE ** (-jnp.arange(half, dtype=f32) / half)
    ang = positions.astype(f32)[..., None] * inv
    cos, sin = jnp.cos(ang), jnp.sin(ang)
    q = jnp.concatenate([q[..., :MLA_NOPE],
                         rotary(q[..., MLA_NOPE:], cos[:, :, None, :], sin[:, :, None, :])], axis=-1)
    k_r = rotary(k_rope, cos, sin)
    k = jnp.concatenate([kv[..., :MLA_NOPE],
                         jnp.broadcast_to(k_r[:, :, None, :], (B, S, MLA_HEADS, MLA_ROPE))], axis=-1)
    v = kv[..., MLA_NOPE:]
    q, k, v = (t.transpose(0, 2, 1, 3) for t in (q, k, v))
    scale = (MLA_NOPE + MLA_ROPE) ** -0.5
    kpos = jnp.arange(S)

    def block(i):
        q0 = i * Q_BLOCK
        qb = lax.dynamic_slice_in_dim(q, q0, Q_BLOCK, axis=2)
        s = jnp.einsum('bhqd,bhkd->bhqk', qb, k, preferred_element_type=f32) * scale
        qpos = q0 + jnp.arange(Q_BLOCK)
        s = jnp.where(kpos[None, :] <= qpos[:, None], s, NEG)
        pr = jax.nn.softmax(s, axis=-1)
        return jnp.einsum('bhqk,bhkd->bhqd', pr.astype(v.dtype), v)

    o = lax.map(block, jnp.arange(S // Q_BLOCK))
    return o.transpose(1, 0, 3, 2, 4).reshape(B, S, MLA_HEADS * MLA_V)


def compress_blocks(x, tok, pos_emb, w1, w2):
    B, H = x.shape[0], x.shape[1]
    blk = x[:, :, tok, :] + pos_emb
    blk = blk.reshape(B, H, tok.shape[0], CMP_LEN * NSA_DIM)
    return jax.nn.gelu(blk @ w1, approximate=True) @ w2


def nsa_attention(q, k_cmp, v_cmp, k_slc, v_slc, k_win, v_win, gate_logits,
                  rel_bias, cmp_pos, cmp_w1, cmp_w2):
    B, S, _ = q.shape
    f32 = jnp.float32
    HK = NSA_KV_HEADS
    G = NSA_HEADS // NSA_KV_HEADS
    D = NSA_DIM
    scale = D ** -0.5
    qg = q.reshape(B, S, HK, G, D).transpose(0, 2, 3, 1, 4)

    def kv_heads(t):
        return t.reshape(B, S, HK, D).transpose(0, 2, 1, 3)

    k_cmp, v_cmp, k_slc, v_slc, k_win, v_win = (
        kv_heads(t) for t in (k_cmp, v_cmp, k_slc, v_slc, k_win, v_win))
    gates = jax.nn.sigmoid(gate_logits.reshape(B, S, HK, G, 3).transpose(0, 2, 3, 1, 4))

    n_cmp = (S - CMP_LEN) // CMP_STRIDE + 1
    cmp_start = jnp.arange(n_cmp) * CMP_STRIDE
    tok = cmp_start[:, None] + jnp.arange(CMP_LEN)[None, :]
    kc = compress_blocks(k_cmp, tok, cmp_pos[0], cmp_w1[0], cmp_w2[0])
    vc = compress_blocks(v_cmp, tok, cmp_pos[1], cmp_w1[1], cmp_w2[1])
    cmp_end = cmp_start + CMP_LEN - 1

    n_slc = S // SLC_LEN
    n_sel = min(SLC_TOPK, n_slc)
    ks_blk = k_slc.reshape(B, HK, n_slc, SLC_LEN, D)
    vs_blk = v_slc.reshape(B, HK, n_slc, SLC_LEN, D)
    slc_start = jnp.arange(n_slc) * SLC_LEN
    overlap = jnp.maximum(
        jnp.minimum(cmp_start[:, None] + CMP_LEN, slc_start[None, :] + SLC_LEN)
        - jnp.maximum(cmp_start[:, None], slc_start[None, :]), 0).astype(f32) / CMP_LEN

    pad = ((0, 0), (0, 0), (WINDOW, 0), (0, 0))
    kw_pad = jnp.pad(k_win, pad)
    vw_pad = jnp.pad(v_win, pad)

    table = rel_bias.astype(f32)
    table_g = table.reshape(REL_BUCKETS, HK, G).transpose(1, 0, 2)

    def head_bias(dist):
        bq = table[rel_bucket(dist)]
        return bq.transpose(2, 0, 1).reshape(HK, G, dist.shape[0], dist.shape[1])

    bi = jnp.arange(B)[:, None, None, None]
    hi = jnp.arange(HK)[None, :, None, None]
    blk_ids = jnp.arange(n_slc)

    def chunk(i):
        q0 = i * Q_BLOCK
        qpos = q0 + jnp.arange(Q_BLOCK)
        qb = lax.dynamic_slice_in_dim(qg, q0, Q_BLOCK, axis=3)
        gb = lax.dynamic_slice_in_dim(gates, q0, Q_BLOCK, axis=3)
        dist_c = qpos[:, None] - cmp_end[None, :]
        s_c = jnp.einsum('bhgqd,bhcd->bhgqc', qb, kc, preferred_element_type=f32) * scale + head_bias(dist_c)
        s_c = jnp.where(dist_c >= 0, s_c, NEG)
        p_c = jax.nn.softmax(s_c, axis=-1) * (qpos >= CMP_LEN - 1)[:, None].astype(f32)
        o_c = jnp.einsum('bhgqc,bhcd->bhgqd', p_c.astype(vc.dtype), vc)
        imp = jnp.einsum('bhgqc,cn->bhqn', p_c, overlap)
        q_blk = qpos // SLC_LEN
        forced = ((blk_ids[None, :] == 0) | (blk_ids[None, :] == q_blk[:, None])
                  | (blk_ids[None, :] == q_blk[:, None] - 1))
        imp = jnp.where(forced, POS_BIG, jnp.where(blk_ids[None, :] > q_blk[:, None], NEG, imp))
        _, sel = lax.top_k(imp, n_sel)
        k_g = ks_blk[bi, hi, sel]
        v_g = vs_blk[bi, hi, sel]
        kpos = sel[..., None] * SLC_LEN + jnp.arange(SLC_LEN)
        dist_s = qpos[:, None, None] - kpos
        bias_s = table_g[hi[..., None], rel_bucket(dist_s)].transpose(0, 1, 5, 2, 3, 4)
        s_s = jnp.einsum('bhgqd,bhqnld->bhgqnl', qb, k_g, preferred_element_type=f32) * scale + bias_s
        s_s = jnp.where((dist_s >= 0)[:, :, None], s_s, NEG)
        p_s = jax.nn.softmax(s_s.reshape(s_s.shape[:4] + (-1,)), axis=-1).reshape(s_s.shape)
        o_s = jnp.einsum('bhgqnl,bhqnld->bhgqd', p_s.astype(v_g.dtype), v_g)
        k_w = lax.dynamic_slice_in_dim(kw_pad, q0, Q_BLOCK + WINDOW, axis=2)
        v_w = lax.dynamic_slice_in_dim(vw_pad, q0, Q_BLOCK + WINDOW, axis=2)
        dist_w = qpos[:, None] - (q0 - WINDOW + jnp.arange(Q_BLOCK + WINDOW))[None, :]
        s_w = jnp.einsum('bhgqd,bhkd->bhgqk', qb, k_w, preferred_element_type=f32) * scale + head_bias(dist_w)
        s_w = jnp.where((dist_w >= 0) & (dist_w < WINDOW), s_w, NEG)
        p_w = jax.nn.softmax(s_w, axis=-1)
        o_w = jnp.einsum('bhgqk,bhkd->bhgqd', p_w.astype(v_w.dtype), v_w)
        out = gb[..., 0:1] * o_c + gb[..., 1:2] * o_s + gb[..., 2:3] * o_w
        return out.astype(q.dtype)

    o = lax.map(chunk, jnp.arange(S // Q_BLOCK))
    return o.transpose(1, 0, 4, 2, 3, 5).reshape(B, S, NSA_HEADS * D)


def conv_ffn(h, w_gate, w_up, conv_w, conv_b, w_down):
    S = h.shape[1]
    g = h @ w_gate
    gp = jnp.pad(g, ((0, 0), (CONV_WIDTH - 1, 0), (0, 0)))
    g = sum(conv_w[k] * gp[:, k:k + S] for k in range(CONV_WIDTH)) + conv_b
    return (jax.nn.gelu(g, approximate=True) * (h @ w_up)) @ w_down


def setup_inputs(seed: int = 0) -> dict:
    key = jax.random.key(seed)
    ks = jax.random.split(key, 26)
    f32 = jnp.float32

    def nrm(k, shape, fan_in):
        return jax.random.normal(k, shape, f32) * fan_in ** -0.5

    def gain(k, shape):
        return 1.0 + 0.05 * jax.random.normal(k, shape, f32)

    n_in = sum(IN_SPLITS)
    return {
        "x": jax.random.normal(ks[0], (BATCH, SEQ, D_MODEL), f32),
        "p": jax.random.normal(ks[1], (DEPTH, BATCH, SEQ, PLE_DIM), f32),
        "positions": jnp.tile(jnp.arange(SEQ, dtype=jnp.int32)[None, :], (BATCH, 1)),
        "rel_bias": 0.5 * jax.random.normal(ks[2], (REL_BUCKETS, NSA_HEADS), f32),
        "attn_pre_norm": gain(ks[3], (DEPTH, D_MODEL)),
        "attn_post_norm": gain(ks[4], (DEPTH, D_MODEL)),
        "ffn_pre_norm": gain(ks[5], (DEPTH, D_MODEL)),
        "ffn_post_norm": gain(ks[6], (DEPTH, D_MODEL)),
        "w_in": nrm(ks[7], (DEPTH, D_MODEL, n_in), D_MODEL),
        "mla_q_norm": gain(ks[8], (DEPTH, MLA_Q_RANK)),
        "mla_w_uq": nrm(ks[9], (DEPTH, MLA_Q_RANK, MLA_HEADS * (MLA_NOPE + MLA_ROPE)), MLA_Q_RANK),
        "mla_kv_norm": gain(ks[10], (DEPTH, MLA_KV_RANK)),
        "mla_w_ukv": nrm(ks[11], (DEPTH, MLA_KV_RANK, MLA_HEADS * (MLA_NOPE + MLA_V)), MLA_KV_RANK),
        "nsa_cmp_pos": 0.1 * jax.random.normal(ks[12], (DEPTH, 2, CMP_LEN, NSA_DIM), f32),
        "nsa_cmp_w1": nrm(ks[13], (DEPTH, 2, CMP_LEN * NSA_DIM, CMP_HIDDEN), CMP_LEN * NSA_DIM),
        "nsa_cmp_w2": nrm(ks[14], (DEPTH, 2, CMP_HIDDEN, NSA_DIM), CMP_HIDDEN),
        "w_o": nrm(ks[15], (DEPTH, MIX_OUT, D_MODEL), MIX_OUT),
        "ffn_w_gate": nrm(ks[16], (DEPTH, D_MODEL, D_FF), D_MODEL),
        "ffn_w_up": nrm(ks[17], (DEPTH, D_MODEL, D_FF), D_MODEL),
        "ffn_conv_w": nrm(ks[18], (DEPTH, CONV_WIDTH, D_FF), CONV_WIDTH),
        "ffn_conv_b": 0.02 * jax.random.normal(ks[19], (DEPTH, D_FF), f32),
        "ffn_w_down": nrm(ks[20], (DEPTH, D_FF, D_MODEL), D_FF),
        "ple_proj": nrm(ks[21], (DEPTH, PLE_DIM, D_MODEL), PLE_DIM),
        "ple_gate": nrm(ks[22], (DEPTH, D_MODEL, D_MODEL), D_MODEL),
    }


def reference(x, p, positions, rel_bias, attn_pre_norm, attn_post_norm, ffn_pre_norm,
              ffn_post_norm, w_in, mla_q_norm, mla_w_uq, mla_kv_norm, mla_w_ukv,
              nsa_cmp_pos, nsa_cmp_w1, nsa_cmp_w2, w_o, ffn_w_gate, ffn_w_up,
              ffn_conv_w, ffn_conv_b, ffn_w_down, ple_proj, ple_gate):
    offsets = np.cumsum(IN_SPLITS)[:-1].tolist()
    for i in range(DEPTH):
        h = rms_norm(x, attn_pre_norm[i])
        z = h @ w_in[i]
        (c_q, c_kv, k_rope, q_nsa, k_cmp, v_cmp, k_slc, v_slc,
         k_win, v_win, g_nsa) = jnp.split(z, offsets, axis=-1)
        o_mla = mla_attention(c_q, c_kv, k_rope, positions, mla_q_norm[i], mla_w_uq[i],
                              mla_kv_norm[i], mla_w_ukv[i])
        o_nsa = nsa_attention(q_nsa, k_cmp, v_cmp, k_slc, v_slc, k_win, v_win, g_nsa,
                              rel_bias, nsa_cmp_pos[i], nsa_cmp_w1[i], nsa_cmp_w2[i])
        y = jnp.concatenate([o_mla, o_nsa], axis=-1) @ w_o[i]
        x = x + rms_norm(y, attn_post_norm[i])
        f = conv_ffn(rms_norm(x, ffn_pre_norm[i]), ffn_w_gate[i], ffn_w_up[i],
                     ffn_conv_w[i], ffn_conv_b[i], ffn_w_down[i])
        x = x + rms_norm(f, ffn_post_norm[i])
        x = x + jax.nn.sigmoid(x @ ple_gate[i]) * (p[i] @ ple_proj[i])
    return x
```

```python
import contextlib
import math
import numpy as np
import concourse.bass as bass
import concourse.mybir as mybir
from concourse.bass_utils import run_bass_kernel_spmd

F32 = mybir.dt.float32
BF16 = mybir.dt.bfloat16
I32 = mybir.dt.int32
AF = mybir.ActivationFunctionType
ALU = mybir.AluOpType

N_DMA_SEMS = 8
USE_POOL_DMA = True
SAME_ENG_SYNC = True
EPS = 1e-6
NEGB = -30000.0


class Res:
    __slots__ = ("lw", "rd_eng", "rd_dma", "pg", "pre")

    def __init__(self):
        self.lw = []
        self.rd_eng = {}
        self.rd_dma = []
        self.pg = None
        self.pre = []


class Op:
    __slots__ = ("eng", "fn", "dma", "deps", "signal", "count", "sem", "val")

    def __init__(self, eng, fn, dma):
        self.eng = eng
        self.fn = fn
        self.dma = dma
        self.deps = ()
        self.signal = False
        self.count = 0
        self.sem = None
        self.val = 0


class Sched:
    ENGS = ("pe", "act", "dve", "pool", "sp")
    OBJ = {"pe": "tensor", "act": "scalar", "dve": "vector", "pool": "gpsimd", "sp": "sync"}

    def __init__(self, nc, st):
        self.nc = nc
        self.q = {e: [] for e in self.ENGS}
        self.esem = {e: st.enter_context(nc.semaphore("c_" + e)) for e in self.ENGS if e != "sp"}
        self.dsem = {e: [st.enter_context(nc.semaphore("d_%s%d" % (e, i))) for i in range(N_DMA_SEMS)]
                     for e in ("sp", "pool")}
        self.ecount = {e: 0 for e in self.ENGS}
        self.dk = {e: 0 for e in ("sp", "pool")}
        self.nops = 0

    mute = False

    def op(self, eng, fn, reads=(), writes=(), dma=False, par=None):
        if self.mute:
            return None
        o = Op(eng, fn, dma)
        self.nops += 1
        deps = {}
        for r in reads:
            for d in r.lw:
                deps[id(d)] = d
        for w in writes:
            if par is not None and w.pg is par:
                for d in w.pre:
                    deps[id(d)] = d
            else:
                cur = list(w.lw) + list(w.rd_eng.values()) + list(w.rd_dma)
                for d in cur:
                    deps[id(d)] = d
                if par is not None:
                    w.pre = cur
        keep = []
        for d in deps.values():
            if d.dma:
                keep.append(d)
            elif d.eng != eng:
                d.signal = True
                keep.append(d)
            elif dma or (SAME_ENG_SYNC and eng != "pe"):
                d.signal = True
                keep.append(d)
        o.deps = keep
        for r in reads:
            if dma:
                r.rd_dma.append(o)
            else:
                r.rd_eng[eng] = o
        for w in writes:
            if par is not None and w.pg is par:
                w.lw.append(o)
            else:
                w.lw = [o]
                w.rd_eng = {}
                w.rd_dma = []
                w.pg = par
        self.q[eng].append(o)
        return o

    def dma(self, eng, out, in_, reads=(), writes=(), par=None):
        return self.op(eng, lambda e: e.dma_start(out=out, in_=in_), reads, writes, dma=True, par=par)

    def mm(self, out, lhsT, rhs, start, stop, reads, writes, skip=False):
        if skip:
            return self.op("pe", lambda e: e.matmul(out, lhsT=lhsT, rhs=rhs, start=start, stop=stop,
                                                    skip_group_check=True), reads, writes)
        return self.op("pe", lambda e: e.matmul(out, lhsT=lhsT, rhs=rhs, start=start, stop=stop), reads, writes)

    def tr(self, out, in_, ident, reads, writes):
        return self.op("pe", lambda e: e.transpose(out=out, in_=in_, identity=ident), reads, writes)

    def act(self, out, in_, func, reads, writes, eng="act", **kw):
        return self.op("act", lambda e: e.activation(out=out, in_=in_, func=func, **kw), reads, writes)

    def cp(self, eng, out, in_, reads, writes):
        if eng == "act":
            return self.op("act", lambda e: e.copy(out=out, in_=in_), reads, writes)
        return self.op(eng, lambda e: e.tensor_copy(out=out, in_=in_), reads, writes)

    def ts(self, eng, out, in0, s1, s2, op0, op1, reads, writes):
        if op1 is None:
            return self.op(eng, lambda e: e.tensor_scalar(out=out, in0=in0, scalar1=s1, scalar2=None, op0=op0),
                           reads, writes)
        return self.op(eng, lambda e: e.tensor_scalar(out=out, in0=in0, scalar1=s1, scalar2=s2, op0=op0, op1=op1),
                       reads, writes)

    def tt(self, eng, out, in0, in1, op, reads, writes):
        return self.op(eng, lambda e: e.tensor_tensor(out=out, in0=in0, in1=in1, op=op), reads, writes)

    def stt(self, out, in0, scalar, in1, op0, op1, reads, writes):
        return self.op("dve", lambda e: e.scalar_tensor_tensor(out=out, in0=in0, scalar=scalar, in1=in1,
                                                               op0=op0, op1=op1), reads, writes)

    def memset(self, eng, out, val, writes):
        return self.op(eng, lambda e: e.memset(out, val), (), writes)

    def flush(self):
        nc = self.nc
        for e in self.ENGS:
            c = self.ecount[e]
            last = None
            for o in self.q[e]:
                if not o.dma:
                    last = o
            if last is not None:
                last.signal = True
            for o in self.q[e]:
                if o.dma:
                    k = self.dk[e]
                    o.sem = self.dsem[e][k % N_DMA_SEMS]
                    o.val = 16 * (k // N_DMA_SEMS + 1)
                    self.dk[e] = k + 1
                elif o.signal:
                    c += 1
                    o.count = c
            self.ecount[e] = c
        fin = []
        for e in self.ENGS:
            if e != "sp":
                fin.append((self.esem[e], self.ecount[e]))
        for e in ("sp", "pool"):
            k = self.dk[e]
            for i in range(N_DMA_SEMS):
                n = (k - i + N_DMA_SEMS - 1) // N_DMA_SEMS if k > i else 0
                fin.append((self.dsem[e][i], 16 * n))
        qs = self.q
        self.q = {e: [] for e in self.ENGS}
        esem = self.esem

        def run(e):
            ops = qs[e]

            def body(eng):
                waited = {}
                for o in ops:
                    ws = []
                    for d in o.deps:
                        if d.dma:
                            ws.append((d.sem, d.val))
                        else:
                            ws.append((esem[d.eng], d.count))
                    if o.dma and o.val > 16:
                        ws.append((o.sem, o.val - 16))
                    for s, v in ws:
                        if waited.get(id(s), 0) < v:
                            eng.wait_ge(s, v)
                            waited[id(s)] = v
                    inst = o.fn(eng)
                    if o.dma:
                        inst.then_inc(o.sem, 16)
                    elif o.signal:
                        inst.then_inc(esem[e], 1)
                for s, v in fin:
                    if v > 0 and waited.get(id(s), 0) < v:
                        eng.wait_ge(s, v)
            return body

        import os as _os
        if _os.environ.get("DUMP"):
            names = {id(v): "c_" + k for k, v in esem.items()}
            for q_, lst in self.dsem.items():
                for i, v in enumerate(lst):
                    names[id(v)] = "d_%s%d" % (q_, i)
            for e in self.ENGS:
                waited = {}
                out = []
                for o in qs[e]:
                    ws = []
                    for d in o.deps:
                        ws.append((d.sem, d.val) if d.dma else (esem[d.eng], d.count))
                    if o.dma and o.val > 16:
                        ws.append((o.sem, o.val - 16))
                    for s_, v in ws:
                        if waited.get(id(s_), 0) < v:
                            out.append("W %s>=%d" % (names[id(s_)], v))
                            waited[id(s_)] = v
                    if o.dma:
                        out.append("DMA +%s=%d" % (names[id(o.sem)], o.val))
                    elif o.signal:
                        out.append("OP +c_%s=%d" % (e, o.count))
                    else:
                        out.append("OP")
                for s_, v in fin:
                    if v > 0 and waited.get(id(s_), 0) < v:
                        out.append("FW %s>=%d" % (names[id(s_)], v))
                print("ENG", e, len(qs[e]), " ".join(out[:400]))
        with nc.Block() as block:
            for e in self.ENGS:
                getattr(block, self.OBJ[e])(run(e))


def _bucket(d):
    d = np.asarray(d)
    n = np.maximum(d, 0)
    large = 16 + (np.log(np.maximum(n, 1).astype(np.float32) / 16) / math.log(128 / 16) * 16).astype(np.int32)
    large = np.minimum(large, 31)
    return np.where(n < 16, n, large)


LV = 416
VOFF = 160


def make_consts(S):
    NT = S // 128
    c = {}
    c["identf"] = np.eye(128, dtype=np.float32)
    k = np.arange(128)[:, None]
    q = np.arange(128)[None, :]
    c["tri"] = (q >= k).astype(np.float32)
    c["wmask"] = (k > q).astype(np.float32)
    d = np.arange(LV) - VOFF
    oh = np.zeros((33, LV), np.float32)
    b = _bucket(d)
    for i in range(LV):
        if d[i] >= 0:
            oh[b[i], i] = 1.0
        else:
            oh[32, i] = 1.0
    c["oh"] = oh
    zz = np.zeros((32, 640), np.float32)
    for j in range(16):
        zz[j, 256 + j] = 1.0
        zz[16 + j, 256 + j] = 1.0
    c["zz"] = zz
    t = np.arange(S)
    c["eblk"] = (t[None, :] // 64 == np.arange(64)[:, None]).astype(np.float32)
    ncmp = (S - 32) // 16 + 1
    cs = np.arange(ncmp) * 16
    ss = np.arange(S // 64) * 64
    ov = np.maximum(np.minimum(cs[:, None] + 32, ss[None, :] + 64) - np.maximum(cs[:, None], ss[None, :]), 0) / 32.0
    ovp = np.zeros((((ncmp + 127) // 128) * 128, 64), np.float32)
    ovp[:ncmp, :S // 64] = ov
    c["ov"] = ovp
    M = np.zeros((NT, 128, 64), np.float32)
    A = np.zeros((NT, 128, 64), np.float32)
    for qt in range(NT):
        qpos = qt * 128 + np.arange(128)
        qb = qpos // 64
        n = np.arange(64)[None, :]
        forced = (n == 0) | (n == qb[:, None]) | (n == qb[:, None] - 1)
        future = n > qb[:, None]
        M[qt] = (~forced & ~future).astype(np.float32)
        A[qt] = np.where(forced, 1e30, np.where(future, -1e30, 0.0))
    c["mtk"] = M
    c["atk"] = A
    p = np.arange(128)
    inv = (10000.0 ** (-(np.arange(16, dtype=np.float32)) / 16)).astype(np.float32)
    c["invf"] = inv[p % 16][:, None].astype(np.float32)
    c["sgn"] = np.where(p % 32 < 16, -1.0, 1.0).astype(np.float32)[:, None]
    xd = np.zeros((128, 4, 8), np.float32)
    xd[:, 0, :] = 384.0
    for qt in range(1, 4):
        xd[:, qt, :] = (127 - np.arange(128) + (3 - qt) * 128)[:, None]
    c["xdst"] = xd.reshape(128, 32)
    return c


CONST_SHAPES = None


def build(S=4096, L=2, dbg=(), stop_after=None, NBC=1):
    NT = S // 128
    NB = S // 512
    NCMP = (S - 32) // 16 + 1
    NCT = (NCMP + 127) // 128
    NBLK = S // 64
    nc = bass.Bass("TRN2", target_bir_lowering=False)
    consts = make_consts(S)

    def din(name, shape, dt=F32):
        return nc.dram_tensor(name, list(shape), dt, kind="ExternalInput")

    def dscr(name, shape, dt=F32):
        kind = "ExternalOutput" if name in dbg else "Internal"
        return nc.dram_tensor(name, list(shape), dt, kind=kind)

    x_in = din("x", [NBC, S, 1024])
    p_in = din("p", [L, NBC, S, 256])
    pos_in = din("pos", [NBC, S], I32)
    relb = din("relb", [32, 8])
    class Lazy(dict):
        def __init__(self, fn):
            super().__init__()
            self.fn = fn

        def __missing__(self, k):
            self[k] = self.fn(k)
            return self[k]

    cd = Lazy(lambda k: din("c_" + k, consts[k].shape))
    WSH = dict(g_pre=[1, 1024], g_post=[1, 1024], g_fpre=[1, 1024], g_fpost=[1, 1024],
               w_fm=[1024, 1024], w_kr=[1024, 64], w_tm=[1024, 664], qn_g=[1, 256], kvn_g=[1, 128],
               wuq_n=[256, 512], wuq_r=[256, 512], wukv_k=[128, 512], wukv_v=[128, 512],
               cmp_pos=[2, 64, 32], cmp_w1=[2, 64, 32 * 128], cmp_w2=[2, 128, 64],
               w_o=[1024, 1024], w_gate=[1024, 2816], w_up=[1024, 2816], conv_w=[128, 22 * 3],
               conv_b=[128, 22], w_down=[2816, 1024], ple_proj=[256, 1024], ple_gate=[1024, 1024])
    W = [Lazy(lambda k, l=l: din("l%d_%s" % (l, k), WSH[k])) for l in range(L)]
    out_d = nc.dram_tensor("out", [NBC, S, 1024], F32, kind="ExternalOutput")

    cs_d = dscr("cs", [2, 128, S])
    rep_d = dscr("rep", [8, 128, LV])
    eb_d = dscr("eb", [2, 2, 128, 512], BF16)
    bc_d = dscr("bc", [2, 32, 512], BF16)
    xden_d = dscr("xden", [128, 32])
    fmT = dscr("fmT", [1024, S], BF16)
    krT = dscr("krT", [32, S], BF16)
    vnsa = dscr("vnsa", [S, 256], BF16)
    gates_d = dscr("gates", [S, 24])
    qnT = dscr("qnT", [512, S], BF16)
    qrT = dscr("qrT", [256, S], BF16)
    knT = dscr("knT", [512, S], BF16)
    vmla = dscr("vmla", [S, 512], BF16)
    kcT = dscr("kcT", [2, 64, NCT * 128], BF16)
    vc_d = dscr("vc", [2, NCT * 128, 64], BF16)
    o_all = dscr("o_all", [S, 1024], BF16)
    xs1 = dscr("xs1", [S, 1024])
    xs2 = dscr("xs2", [S, 1024])
    xs3 = dscr("xs3", [S, 1024])
    aT_d = dscr("aT", [2816, S], BF16)

    with contextlib.ExitStack() as top:
        Sc = Sched(nc, top)
        rr = [0]

        def ev_eng():
            rr[0] += 1
            return "act" if rr[0] % 2 else "dve"

        def dq():
            rr[0] += 1
            return "sp" if (rr[0] % 2 or not USE_POOL_DMA) else "pool"

        def dma_split(eng, out_ap, in_ap, axis, step, reads=(), writes=()):
            n = out_ap.shape[axis]
            tok = object()
            for a in range(0, n, step):
                b = min(n, a + step)
                idx = [slice(None)] * len(out_ap.shape)
                idx[axis] = slice(a, b)
                Sc.dma(eng, out_ap[tuple(idx)], in_ap[tuple(idx)], reads=reads, writes=writes, par=tok)

        class Scope:
            def __init__(self):
                self.st = contextlib.ExitStack()
                self.n = 0

            def __enter__(self):
                self.st.__enter__()
                return self

            def __exit__(self, *a):
                Sc.flush()
                return self.st.__exit__(*a)

            def sb(self, shape, dt=F32):
                self.n += 1
                t = self.st.enter_context(nc.sbuf_tensor("t%d_%d" % (Sc.nops, self.n), list(shape), dt))
                return t, Res()

            def ps(self, shape=(128, 512), dt=F32):
                self.n += 1
                t = self.st.enter_context(nc.psum_tensor("p%d_%d" % (Sc.nops, self.n), list(shape), dt))
                return t, Res()

        def bcast_rows(dram, row0, nrows_p, ncols):
            return bass.AP(dram, row0 * ncols, [[0, nrows_p], [1, ncols]])

        def load_cast(sc, dst, rdst, src_ap, shape, eng=None, stage=None):
            if stage is None:
                stage = sc.sb(shape, F32)
            stg, rstg = stage
            sv = stg[:]
            if len(src_ap.shape) == 3:
                if tuple(sv.shape) != tuple(src_ap.shape):
                    sv = stg[:, 0:src_ap.shape[1], 0:src_ap.shape[2]]
                dma_split(dq(), sv, src_ap, 1, 1, writes=[rstg])
            else:
                Sc.dma(dq(), sv, src_ap, writes=[rstg])
            Sc.cp(eng or ev_eng(), dst, sv, [rstg], [rdst])

        def rms_rstd(sc, src_ap, n, junk, rjunk, reads):
            ss, rss = sc.sb([128, 1])
            Sc.act(junk, src_ap, AF.Square, reads, [rjunk, rss], accum_out=ss[:])
            Sc.ts("dve", ss[:], ss[:], 1.0 / n, EPS, ALU.mult, ALU.add, [rss], [rss])
            Sc.act(ss[:], ss[:], AF.Sqrt, [rss], [rss])
            Sc.op("dve", lambda e: e.reciprocal(out=ss[:], in_=ss[:]), [rss], [rss])
            return ss, rss

        for bi_ in range(NBC):
            import os as _os0
            with (Scope() if not _os0.environ.get("SKIP_P0") else contextlib.nullcontext()) as sc:
              if sc is not None:
                  posi, r_posi = sc.sb([128, S], I32)
                  ang, r_ang = sc.sb([128, S])
                  u, r_u = sc.sb([128, S])
                  ki, r_ki = sc.sb([128, S], I32)
                  m, r_m = sc.sb([128, S])
                  invf, r_invf = sc.sb([128, 1])
                  sgn, r_sgn = sc.sb([128, 1])
                  Sc.dma("sp", posi[:], bcast_rows(pos_in, bi_, 128, S), writes=[r_posi])
                  Sc.dma("sp", invf[:], cd["invf"].ap(), writes=[r_invf])
                  Sc.dma("sp", sgn[:], cd["sgn"].ap(), writes=[r_sgn])
                  Sc.cp("dve", ang[:], posi[:], [r_posi], [r_ang])
                  Sc.ts("dve", ang[:], ang[:], invf[:, 0:1], None, ALU.mult, None, [r_ang, r_invf], [r_ang])
                  TWO_PI = 2.0 * math.pi
                  for which in range(2):
                      shift = math.pi / 2 if which == 0 else 0.0
                      Sc.ts("dve", u[:], ang[:], shift, 1.0 / TWO_PI, ALU.add, ALU.mult, [r_ang], [r_u])
                      Sc.cp("dve", ki[:], u[:], [r_u], [r_ki])
                      Sc.cp("dve", m[:], ki[:], [r_ki], [r_m])
                      Sc.stt(u[:], m[:], -TWO_PI, ang[:], ALU.mult, ALU.add, [r_m, r_ang], [r_u])
                      if shift:
                          Sc.ts("dve", u[:], u[:], shift, None, ALU.add, None, [r_u], [r_u])
                      Sc.ts("dve", m[:], u[:], math.pi, None, ALU.is_gt, None, [r_u], [r_m])
                      Sc.stt(u[:], m[:], -TWO_PI, u[:], ALU.mult, ALU.add, [r_m, r_u], [r_u])
                      Sc.ts("dve", m[:], u[:], -math.pi, None, ALU.is_lt, None, [r_u], [r_m])
                      Sc.stt(u[:], m[:], TWO_PI, u[:], ALU.mult, ALU.add, [r_m, r_u], [r_u])
                      Sc.ts("dve", u[:], u[:], -3.1415925, 3.1415925, ALU.max, ALU.min, [r_u], [r_u])
                      Sc.act(m[:], u[:], AF.Sin, [r_u], [r_m])
                      if which == 1:
                          Sc.ts("dve", m[:], m[:], sgn[:, 0:1], None, ALU.mult, None, [r_m, r_sgn], [r_m])
                      Sc.dma("sp", cs_d.ap()[which], m[:], reads=[r_m])

                  P0S = _os0.environ.get('P0_STOP', '')
                  Sc.mute = (P0S == 'a')
                  tb, r_tb = sc.sb([33, 8])
                  t31, r_t31 = sc.sb([32, 8])
                  Sc.memset("dve", tb[:], -4000.0, [r_tb])
                  oh, r_oh = sc.sb([33, LV])
                  pv, r_pv = sc.ps([128, 512])
                  sv, r_sv = sc.sb([128, LV])
                  r_rep = Res()
                  Sc.dma("sp", tb[0:32, :], relb.ap(), writes=[r_tb])
                  Sc.dma("sp", t31[:], bcast_rows(relb, 31, 32, 8), writes=[r_t31])
                  Sc.dma("sp", oh[:], cd["oh"].ap(), writes=[r_oh])
                  Sc.tt("dve", tb[0:32, :], tb[0:32, :], t31[:], ALU.subtract, [r_tb, r_t31], [r_tb])
                  ohb, r_ohb = sc.sb([33, LV], BF16)
                  Sc.cp("dve", ohb[:], oh[:], [r_oh], [r_ohb])
                  tbh, r_tbh = sc.sb([33, 8], BF16)
                  tbf, r_tbf = sc.sb([33, 8])
                  tbl_, r_tbl = sc.sb([33, 8], BF16)
                  Sc.cp("dve", tbh[:], tb[:], [r_tb], [r_tbh])
                  Sc.cp("dve", tbf[:], tbh[:], [r_tbh], [r_tbf])
                  Sc.tt("dve", tbf[:], tb[:], tbf[:], ALU.subtract, [r_tb, r_tbf], [r_tbf])
                  Sc.cp("dve", tbl_[:], tbf[:], [r_tbf], [r_tbl])
                  lwh, r_lwh = sc.sb([33, 128], BF16)
                  lwl, r_lwl = sc.sb([33, 128], BF16)
                  for h in range(8):
                      Sc.cp("dve", lwh[:], tbh[:, h:h + 1].to_broadcast([33, 128]), [r_tbh], [r_lwh])
                      Sc.cp("dve", lwl[:], tbl_[:, h:h + 1].to_broadcast([33, 128]), [r_tbl], [r_lwl])
                      Sc.mm(pv[:, 0:LV], lwh[:], ohb[:], True, False, [r_lwh, r_ohb], [r_pv])
                      Sc.mm(pv[:, 0:LV], lwl[:], ohb[:], False, True, [r_lwl, r_ohb], [r_pv])
                      Sc.cp("act", sv[:], pv[:, 0:LV], [r_pv], [r_sv])
                      Sc.dma("sp", rep_d.ap()[h], sv[:], reads=[r_sv], writes=[r_rep])
                  Sc.mute = Sc.mute or (P0S == 'b')
                  wmf, r_wmf = sc.sb([128, 128])
                  wmb, r_wmb = sc.sb([128, 128], BF16)
                  xd, r_xd = sc.sb([128, 32])
                  e0, r_e0 = sc.sb([128, 8])
                  e0h, r_e0h = sc.sb([128, 8], BF16)
                  e0f, r_e0f = sc.sb([128, 8])
                  e0l, r_e0l = sc.sb([128, 8], BF16)
                  Sc.dma("sp", wmf[:], cd["wmask"].ap(), writes=[r_wmf])
                  Sc.cp("dve", wmb[:], wmf[:], [r_wmf], [r_wmb])
                  Sc.dma("sp", xd[:], cd["xdst"].ap(), writes=[r_xd])
                  Sc.mm(pv[:, 0:8], ohb[:, VOFF:VOFF + 128], tbh[:], True, False, [r_ohb, r_tbh], [r_pv])
                  Sc.mm(pv[:, 0:8], ohb[:, VOFF:VOFF + 128], tbl_[:], False, True, [r_ohb, r_tbl], [r_pv])
                  Sc.act(e0[:], pv[:, 0:8], AF.Exp, [r_pv], [r_e0])
                  Sc.cp("dve", e0h[:], e0[:], [r_e0], [r_e0h])
                  Sc.cp("dve", e0f[:], e0h[:], [r_e0h], [r_e0f])
                  Sc.tt("dve", e0f[:], e0[:], e0f[:], ALU.subtract, [r_e0, r_e0f], [r_e0f])
                  Sc.cp("dve", e0l[:], e0f[:], [r_e0f], [r_e0l])
                  Sc.mm(pv[:, 0:8], wmb[:], e0h[:], True, False, [r_wmb, r_e0h], [r_pv])
                  Sc.mm(pv[:, 0:8], wmb[:], e0l[:], False, True, [r_wmb, r_e0l], [r_pv])
                  Sc.tt("dve", xd[:, 0:8], xd[:, 0:8], pv[:, 0:8], ALU.add, [r_xd, r_pv], [r_xd])
                  Sc.dma("sp", xden_d.ap(), xd[:], reads=[r_xd])
                  Sc.mute = Sc.mute or (P0S == 'c')
                  tri, r_tri = sc.sb([128, 128])
                  Sc.dma("sp", tri[:], cd["tri"].ap(), writes=[r_tri])
                  for dl in range(2):
                      for hk in range(2):
                          tl, r_tl = sc.sb([128, 512])
                          eb, r_eb = sc.sb([128, 512], BF16)
                          for g in range(4):
                              h = hk * 4 + g
                              src = bass.AP(rep_d, h * 128 * LV + 128 * dl + VOFF, [[LV - 1, 128], [1, 128]])
                              Sc.dma("sp", tl[:, g * 128:(g + 1) * 128], src, reads=[r_rep], writes=[r_tl])
                          Sc.act(tl[:], tl[:], AF.Exp, [r_tl], [r_tl])
                          if dl == 0:
                              for g in range(4):
                                  Sc.tt("dve", tl[:, g * 128:(g + 1) * 128], tl[:, g * 128:(g + 1) * 128], tri[:],
                                        ALU.mult, [r_tl, r_tri], [r_tl])
                          Sc.cp("dve", eb[:], tl[:], [r_tl], [r_eb])
                          Sc.dma("sp", eb_d.ap()[dl, hk], eb[:], reads=[r_eb])
                  for hk in range(2):
                      bt, r_bt = sc.sb([16, 512])
                      bhi, r_bhi = sc.sb([16, 512], BF16)
                      bhf, r_bhf = sc.sb([16, 512])
                      blo, r_blo = sc.sb([16, 512], BF16)
                      for g in range(4):
                          h = hk * 4 + g
                          src = bass.AP(rep_d, h * 128 * LV + 97 + VOFF, [[LV - 16, 16], [1, 128]])
                          Sc.dma("sp", bt[:, g * 128:(g + 1) * 128], src, reads=[r_rep], writes=[r_bt])
                      Sc.ts("dve", bt[:], bt[:], 8.0, None, ALU.mult, None, [r_bt], [r_bt])
                      Sc.cp("dve", bhi[:], bt[:], [r_bt], [r_bhi])
                      Sc.cp("dve", bhf[:], bhi[:], [r_bhi], [r_bhf])
                      Sc.tt("dve", bhf[:], bt[:], bhf[:], ALU.subtract, [r_bt, r_bhf], [r_bhf])
                      Sc.cp("dve", blo[:], bhf[:], [r_bhf], [r_blo])
                      Sc.dma("sp", bc_d.ap()[hk, 0:16, :], bhi[:], reads=[r_bhi])
                      Sc.dma("sp", bc_d.ap()[hk, 16:32, :], blo[:], reads=[r_blo])
            Sc.mute = False
            if stop_after == "P0":
                return nc

            x_src = x_in.ap()[bi_]
            for l in range(L):
                Wl = W[l]
                x_dst = out_d.ap()[bi_] if l == L - 1 else xs3.ap()
                with Scope() as sc:
                    identf, r_idf = sc.sb([128, 128])
                    identb, r_idb = sc.sb([128, 128], BF16)
                    Sc.dma("sp", identf[:], cd["identf"].ap(), writes=[r_idf])
                    Sc.cp("dve", identb[:], identf[:], [r_idf], [r_idb])
                    gpre, r_gpre = sc.sb([128, 1024])
                    qng, r_qng = sc.sb([128, 256])
                    kvng, r_kvng = sc.sb([128, 128])
                    Sc.dma("sp", gpre[:], bcast_rows(Wl["g_pre"], 0, 128, 1024), writes=[r_gpre])
                    Sc.dma("sp", qng[:], bcast_rows(Wl["qn_g"], 0, 128, 256), writes=[r_qng])
                    Sc.dma("sp", kvng[:], bcast_rows(Wl["kvn_g"], 0, 128, 128), writes=[r_kvng])
                    stage = sc.sb([128, 8, 1024])
                    wfm, r_wfm = sc.sb([128, 8, 1024], BF16)
                    load_cast(sc, wfm[:], r_wfm, Wl["w_fm"].ap().rearrange("(k p) n -> p k n", p=128), None, stage=stage)
                    wtm, r_wtm = sc.sb([128, 8, 664], BF16)
                    load_cast(sc, wtm[:], r_wtm, Wl["w_tm"].ap().rearrange("(k p) n -> p k n", p=128), [128, 8, 664])
                    wkr, r_wkr = sc.sb([128, 8, 64], BF16)
                    load_cast(sc, wkr[:], r_wkr, Wl["w_kr"].ap().rearrange("(k p) n -> p k n", p=128), [128, 8, 64])
                    wuqn, r_wuqn = sc.sb([128, 2, 512], BF16)
                    load_cast(sc, wuqn[:], r_wuqn, Wl["wuq_n"].ap().rearrange("(k p) n -> p k n", p=128), [128, 2, 512])
                    wuqr, r_wuqr = sc.sb([128, 2, 512], BF16)
                    load_cast(sc, wuqr[:], r_wuqr, Wl["wuq_r"].ap().rearrange("(k p) n -> p k n", p=128), [128, 2, 512])
                    wukk, r_wukk = sc.sb([128, 512], BF16)
                    load_cast(sc, wukk[:], r_wukk, Wl["wukv_k"].ap(), [128, 512])
                    wukv, r_wukv = sc.sb([128, 512], BF16)
                    load_cast(sc, wukv[:], r_wukv, Wl["wukv_v"].ap(), [128, 512])

                    xt = [sc.sb([128, 1024]) for _ in range(2)]
                    junk, r_junk = sc.sb([128, 1024], BF16)
                    hb = [sc.sb([128, 1024], BF16) for _ in range(2)]
                    ptr = [sc.ps([128, 1024], BF16) for _ in range(2)]
                    hT = [sc.sb([128, 8, 512], BF16) for _ in range(2)]
                    pmm = [sc.ps() for _ in range(4)]
                    pq = sc.ps([128, 1024], BF16)
                    c1 = [sc.sb([128, 384]) for _ in range(2)]
                    fo = [sc.sb([128, 512], BF16) for _ in range(3)]
                    cost, r_cost = sc.sb([128, 512])
                    sint, r_sint = sc.sb([128, 512])
                    t1 = [sc.sb([128, 512]) for _ in range(2)]
                    cqn = [sc.sb([128, 384], BF16) for _ in range(2)]
                    cqnT, r_cqnT = sc.sb([128, 2, 512], BF16)
                    ckvnT, r_ckvnT = sc.sb([128, 512], BF16)
                    vo = [sc.sb([128, 256], BF16) for _ in range(2)]
                    go = [sc.sb([128, 24]) for _ in range(2)]
                    vm = [sc.sb([128, 512], BF16) for _ in range(2)]
                    nfo = [0]

                    def evac_store(psum_ap, rps, dst_ap, np_=128):
                        f, rf = fo[nfo[0] % 3]
                        nfo[0] += 1
                        Sc.cp(ev_eng(), f[0:np_, :], psum_ap, [rps], [rf])
                        Sc.dma(dq(), dst_ap, f[0:np_, :], reads=[rf])

                    def rope_combine(pa, rpa, pb, rpb, np_, dst_ap):
                        (a, ra), (b, rb) = t1
                        Sc.tt("dve", a[0:np_, :], pa[0:np_, :], cost[0:np_, :], ALU.mult, [rpa, r_cost], [ra])
                        Sc.tt("dve", b[0:np_, :], pb[0:np_, :], sint[0:np_, :], ALU.mult, [rpb, r_sint], [rb])
                        f, rf = fo[nfo[0] % 3]
                        nfo[0] += 1
                        Sc.tt("pool", f[0:np_, :], a[0:np_, :], b[0:np_, :], ALU.add, [ra, rb], [rf])
                        Sc.dma(dq(), dst_ap, f[0:np_, :], reads=[rf])

                    import os as _os
                    A_STOP = _os.environ.get("A_STOP", "")
                    for tb_ in range(NB if A_STOP != "w" else 0):
                        c0 = tb_ * 512
                        hTt, r_hT = hT[tb_ % 2]
                        Sc.dma("sp", cost[:], cs_d.ap()[0, :, c0:c0 + 512], writes=[r_cost])
                        Sc.dma(dq(), sint[:], cs_d.ap()[1, :, c0:c0 + 512], writes=[r_sint])
                        for i in range(4):
                            ti = tb_ * 4 + i
                            (x_t, r_x) = xt[ti % 2]
                            (h_b, r_hb) = hb[ti % 2]
                            (p_t, r_pt) = ptr[ti % 2]
                            Sc.dma(dq(), x_t[:], x_src[ti * 128:(ti + 1) * 128, :], writes=[r_x])
                            ss, rss = rms_rstd(sc, x_t[:], 1024, junk[:], r_junk, [r_x])
                            Sc.stt(h_b[:], x_t[:], ss[:, 0:1], gpre[:], ALU.mult, ALU.mult, [r_x, rss, r_gpre], [r_hb])
                            for k in range(8):
                                Sc.tr(p_t[:, k * 128:(k + 1) * 128], h_b[:, k * 128:(k + 1) * 128], identb[:],
                                      [r_hb, r_idb], [r_pt])
                            Sc.cp("act", hTt[:, :, i * 128:(i + 1) * 128],
                                  p_t[:].rearrange("p (k t) -> p k t", k=8), [r_pt], [r_hT])
                        if A_STOP == "h":
                            continue
                        for c in range(8):
                            pm, rpm = pmm[c % 2]
                            for k in range(8):
                                Sc.mm(pm[:], wfm[:, k, c * 128:(c + 1) * 128], hTt[:, k, :], k == 0, k == 7,
                                      [r_wfm, r_hT], [rpm])
                            evac_store(pm[:], rpm, fmT.ap()[c * 128:(c + 1) * 128, c0:c0 + 512])
                        if A_STOP == "f1":
                            continue
                        (pa, rpa), (pb, rpb) = pmm[2], pmm[3]
                        for k in range(8):
                            Sc.mm(pa[0:32, :], wkr[:, k, 0:32], hTt[:, k, :], k == 0, k == 7, [r_wkr, r_hT], [rpa])
                        for k in range(8):
                            Sc.mm(pb[0:32, :], wkr[:, k, 32:64], hTt[:, k, :], k == 0, k == 7, [r_wkr, r_hT], [rpb])
                        rope_combine(pa, rpa, pb, rpb, 32, krT.ap()[:, c0:c0 + 512])
                        if A_STOP == "f":
                            continue
                        for i in range(4):
                            ti = tb_ * 4 + i
                            (p1, rp1), (p2, rp2) = pmm[0], pmm[1]
                            T2M = _os.environ.get("T2M", "")
                            for k in range(8 if T2M != "nop1" else 0):
                                Sc.mm(p1[:, 0:384], hTt[:, k, i * 128:(i + 1) * 128], wtm[:, k, 0:384], k == 0, k == 7,
                                      [r_hT, r_wtm], [rp1])
                            for k in range(8 if T2M != "nop2" else 0):
                                Sc.mm(p2[:, 0:280], hTt[:, k, i * 128:(i + 1) * 128], wtm[:, k, 384:664], k == 0, k == 7,
                                      [r_hT, r_wtm], [rp2])
                            if A_STOP == "t2":
                                continue
                            cq, r_cq = cqn[ti % 2]
                            c_1, r_c1 = c1[ti % 2]
                            T1M = int(_os.environ.get("T1M", "9"))
                            Sc.cp("act", c_1[:], p1[:, 0:384], [rp1], [r_c1])
                            Sc.mute = T1M <= 1
                            ss, rss = rms_rstd(sc, c_1[:, 0:256], 256, junk[:, 0:256], r_junk, [r_c1])
                            Sc.stt(cq[:, 0:256], c_1[:, 0:256], ss[:, 0:1], qng[:], ALU.mult, ALU.mult,
                                   [r_c1, rss, r_qng], [r_cq])
                            Sc.mute = T1M <= 2
                            ss2, rss2 = rms_rstd(sc, c_1[:, 256:384], 128, junk[:, 0:128], r_junk, [r_c1])
                            Sc.stt(cq[:, 256:384], c_1[:, 256:384], ss2[:, 0:1], kvng[:], ALU.mult, ALU.mult,
                                   [r_c1, rss2, r_kvng], [r_cq])
                            pqt, rpq = pq
                            Sc.mute = T1M <= 3
                            for k in range(3):
                                Sc.tr(pqt[:, k * 128:(k + 1) * 128], cq[:, k * 128:(k + 1) * 128], identb[:],
                                      [r_cq, r_idb], [rpq])
                            Sc.mute = T1M <= 4
                            Sc.cp("act", cqnT[:, :, i * 128:(i + 1) * 128],
                                  pqt[:, 0:256].rearrange("p (k t) -> p k t", k=2), [rpq], [r_cqnT])
                            Sc.mute = T1M <= 5
                            Sc.cp("act", ckvnT[:, i * 128:(i + 1) * 128], pqt[:, 256:384], [rpq], [r_ckvnT])
                            Sc.mute = False
                            if A_STOP == "t1":
                                continue
                            v_o, r_vo = vo[ti % 2]
                            Sc.cp("dve", v_o[:], p2[:, 0:256], [rp2], [r_vo])
                            Sc.dma(dq(), vnsa.ap()[ti * 128:(ti + 1) * 128, :], v_o[:], reads=[r_vo])
                            g_o, r_go = go[ti % 2]
                            Sc.act(g_o[:], p2[:, 256:280], AF.Sigmoid, [rp2], [r_go])
                            Sc.dma(dq(), gates_d.ap()[ti * 128:(ti + 1) * 128, :], g_o[:], reads=[r_go])
                        if A_STOP in ("t", "t1", "t2"):
                            continue
                        for hp in range(4):
                            pm, rpm = pmm[hp % 2]
                            for k in range(2):
                                Sc.mm(pm[:], wuqn[:, k, hp * 128:(hp + 1) * 128], cqnT[:, k, :], k == 0, k == 1,
                                      [r_wuqn, r_cqnT], [rpm])
                            evac_store(pm[:], rpm, qnT.ap()[hp * 128:(hp + 1) * 128, c0:c0 + 512])
                        for grp in range(2):
                            (pa, rpa), (pb, rpb) = pmm[2], pmm[3]
                            for k in range(2):
                                Sc.mm(pa[:], wuqr[:, k, grp * 128:(grp + 1) * 128], cqnT[:, k, :], k == 0, k == 1,
                                      [r_wuqr, r_cqnT], [rpa])
                            for k in range(2):
                                Sc.mm(pb[:], wuqr[:, k, 256 + grp * 128:256 + (grp + 1) * 128], cqnT[:, k, :], k == 0,
                                      k == 1, [r_wuqr, r_cqnT], [rpb])
                            rope_combine(pa, rpa, pb, rpb, 128, qrT.ap()[grp * 128:(grp + 1) * 128, c0:c0 + 512])
                        for hp in range(4):
                            pm, rpm = pmm[hp % 2]
                            Sc.mm(pm[:], wukk[:, hp * 128:(hp + 1) * 128], ckvnT[:], True, True, [r_wukk, r_ckvnT], [rpm])
                            evac_store(pm[:], rpm, knT.ap()[hp * 128:(hp + 1) * 128, c0:c0 + 512])
                        for i in range(4):
                            ti = tb_ * 4 + i
                            pm, rpm = pmm[i % 2]
                            Sc.mm(pm[:], ckvnT[:, i * 128:(i + 1) * 128], wukv[:], True, True, [r_ckvnT, r_wukv], [rpm])
                            v_m, r_vm = vm[ti % 2]
                            Sc.cp(ev_eng(), v_m[:], pm[:], [rpm], [r_vm])
                            Sc.dma(dq(), vmla.ap()[ti * 128:(ti + 1) * 128, :], v_m[:], reads=[r_vm])
                if stop_after == "A%d" % l:
                    return nc

                with Scope() as sc:
                    w1s, r_w1s = sc.sb([64, 32 * 128])
                    w1b_ = [sc.sb([64, 32, 128], BF16) for _ in range(2)]
                    w2s, r_w2s = sc.sb([128, 64])
                    w2b_ = [sc.sb([128, 64], BF16) for _ in range(2)]
                    pos, r_pos = sc.sb([64, 32])
                    posr_ = [sc.sb([64, 32, NCMP], BF16) for _ in range(2)]
                    xT_ = [sc.sb([64, S], BF16) for _ in range(2)]
                    hid_ = [sc.sb([128, NCT * 128], BF16) for _ in range(2)]
                    ph_ = [sc.ps() for _ in range(2)]
                    po_ = [sc.ps() for _ in range(2)]
                    ko, r_ko = sc.sb([64, NCT * 128], BF16)
                    vo_l = [sc.sb([128, 64], BF16) for _ in range(2)]
                    nb_ = 0
                    for kv in range(2):
                        w1b, r_w1b = w1b_[kv]
                        w2b, r_w2b = w2b_[kv]
                        posr, r_posr = posr_[kv]
                        Sc.dma("sp", w1s[:], Wl["cmp_w1"].ap()[kv], writes=[r_w1s])
                        Sc.cp("dve", w1b[:], w1s[:].rearrange("p (l j) -> p l j", l=32), [r_w1s], [r_w1b])
                        Sc.dma("sp", w2s[:], Wl["cmp_w2"].ap()[kv], writes=[r_w2s])
                        Sc.cp("dve", w2b[:], w2s[:], [r_w2s], [r_w2b])
                        Sc.dma("sp", pos[:], Wl["cmp_pos"].ap()[kv], writes=[r_pos])
                        Sc.cp("pool", posr[:], pos[:].unsqueeze(2).to_broadcast([64, 32, NCMP]), [r_pos], [r_posr])
                        for hk in range(2):
                            xT, r_xT = xT_[hk]
                            hid, r_hid = hid_[hk]
                            ph, rph = ph_[hk]
                            r0 = 512 + kv * 128 + hk * 64
                            Sc.dma("sp", xT[:], fmT.ap()[r0:r0 + 64, :], writes=[r_xT])
                            for li in range(32):
                                rhs = xT[:, li:li + 16 * (NCMP - 1) + 1:16]
                                Sc.mm(ph[:, 0:NCMP], w1b[:, li, :], rhs, li == 0, False, [r_w1b, r_xT], [rph])
                                Sc.mm(ph[:, 0:NCMP], w1b[:, li, :], posr[:, li, :], False, li == 31, [r_w1b, r_posr], [rph])
                            Sc.memset("dve", hid[:], 0.0, [r_hid])
                            Sc.act(hid[:, 0:NCMP], ph[:, 0:NCMP], AF.Gelu_apprx_tanh, [rph], [r_hid])
                            if kv == 0:
                                po, rpo = po_[nb_ % 2]
                                nb_ += 1
                                Sc.mm(po[0:64, 0:NCT * 128], w2b[:], hid[:], True, True, [r_w2b, r_hid], [rpo])
                                Sc.cp("dve", ko[:], po[0:64, 0:NCT * 128], [rpo], [r_ko])
                                Sc.dma("sp", kcT.ap()[hk], ko[:], reads=[r_ko])
                            else:
                                for ct in range(NCT):
                                    po, rpo = po_[nb_ % 2]
                                    vo_, r_vo_ = vo_l[nb_ % 2]
                                    nb_ += 1
                                    Sc.mm(po[:, 0:64], hid[:, ct * 128:(ct + 1) * 128], w2b[:], True, True,
                                          [r_hid, r_w2b], [rpo])
                                    Sc.cp("dve", vo_[:], po[:, 0:64], [rpo], [r_vo_])
                                    Sc.dma("sp", vc_d.ap()[hk, ct * 128:(ct + 1) * 128, :], vo_[:], reads=[r_vo_])
                if stop_after == "B%d" % l:
                    return nc

                for hk in range(2):
                    with Scope() as sc:
                        identf, r_idf = sc.sb([128, 128])
                        Sc.dma("sp", identf[:], cd["identf"].ap(), writes=[r_idf])
                        ksT, r_ksT = sc.sb([128, S], BF16)
                        Sc.dma("sp", ksT[0:64, :], fmT.ap()[768 + hk * 64:768 + hk * 64 + 64, :], writes=[r_ksT])
                        est, r_est = sc.sb([128, S])
                        Sc.dma(dq(), est[64:128, :], cd["eblk"].ap(), writes=[r_est])
                        Sc.cp("dve", ksT[64:128, :], est[64:128, :], [r_est], [r_ksT])
                        kwT, r_kwT = sc.sb([64, S], BF16)
                        Sc.dma("sp", kwT[:], fmT.ap()[896 + hk * 64:896 + hk * 64 + 64, :], writes=[r_kwT])
                        kc, r_kc = sc.sb([64, NCT * 128], BF16)
                        Sc.dma("sp", kc[:], kcT.ap()[hk], writes=[r_kc])
                        vs, r_vs = sc.sb([128, NT, 65], BF16)
                        vw, r_vw = sc.sb([128, NT, 65], BF16)
                        Sc.memset("dve", vs[:], 1.0, [r_vs])
                        Sc.memset("dve", vw[:], 1.0, [r_vw])
                        dma_split("sp", vs[:, :, 0:64],
                                  vnsa.ap()[:, hk * 64:hk * 64 + 64].rearrange("(t p) d -> p t d", p=128), 1, 4,
                                  writes=[r_vs])
                        dma_split("sp", vw[:, :, 0:64],
                                  vnsa.ap()[:, 128 + hk * 64:128 + hk * 64 + 64].rearrange("(t p) d -> p t d", p=128),
                                  1, 4, writes=[r_vw])
                        vce, r_vce = sc.sb([128, NCT, 129], BF16)
                        Sc.memset("dve", vce[:], 1.0, [r_vce])
                        Sc.dma("sp", vce[:, :, 0:64], vc_d.ap()[hk].rearrange("(t p) d -> p t d", p=128), writes=[r_vce])
                        ovs, r_ovs = sc.sb([128, NCT, 64])
                        Sc.dma("sp", ovs[:], cd["ov"].ap().rearrange("(t p) d -> p t d", p=128), writes=[r_ovs])
                        Sc.cp("dve", vce[:, :, 65:129], ovs[:], [r_ovs], [r_vce])
                        eb0, r_eb0 = sc.sb([128, 512], BF16)
                        eb1, r_eb1 = sc.sb([128, 512], BF16)
                        Sc.dma("sp", eb0[:], eb_d.ap()[0, hk], writes=[r_eb0])
                        Sc.dma("sp", eb1[:], eb_d.ap()[1, hk], writes=[r_eb1])
                        wms, r_wms = sc.sb([128, 128])
                        wm, r_wm = sc.sb([128, 512], BF16)
                        Sc.dma("sp", wms[:], cd["wmask"].ap(), writes=[r_wms])
                        for g in range(4):
                            Sc.cp("dve", wm[:, g * 128:(g + 1) * 128], wms[:], [r_wms], [r_wm])
                        zzs, r_zzs = sc.sb([32, 640])
                        zz, r_zz = sc.sb([32, 640], BF16)
                        Sc.dma("sp", zzs[:], cd["zz"].ap(), writes=[r_zzs])
                        Sc.cp("dve", zz[:], zzs[:], [r_zzs], [r_zz])
                        bc, r_bc = sc.sb([32, 512], BF16)
                        Sc.dma("sp", bc[:], bc_d.ap()[hk], writes=[r_bc])
                        xdn, r_xdn = sc.sb([128, 32])
                        Sc.dma("sp", xdn[:], xden_d.ap(), writes=[r_xdn])

                        qa = [sc.sb([128, 512], BF16) for _ in range(2)]
                        gt = [sc.sb([128, 24]) for _ in range(2)]
                        mt = [sc.sb([128, 64]) for _ in range(2)]
                        at = [sc.sb([128, 64]) for _ in range(2)]
                        pS = [sc.ps() for _ in range(3)]
                        pc = [sc.ps() for _ in range(2)]
                        po_s, rpo_s = sc.ps()
                        po_w, rpo_w = sc.ps()
                        ptT, rptT = sc.ps([128, 1024], BF16)
                        pT = [sc.sb([128, 512], BF16) for _ in range(3)]
                        oacc = [sc.sb([128, 4, 64]) for _ in range(2)]
                        obf = [sc.sb([128, 256], BF16) for _ in range(2)]
                        imp, r_imp = sc.sb([128, 64])
                        imod, r_imod = sc.sb([128, 64])
                        w1_, r_w1_ = sc.sb([128, 64])
                        w2_, r_w2_ = sc.sb([128, 64])
                        m8, r_m8 = sc.sb([128, 8])
                        selb, r_selb = sc.sb([128, 128], BF16)
                        Sc.memset("dve", selb[:], 0.0, [r_selb])
                        identb, r_idb = sc.sb([128, 128], BF16)
                        Sc.cp("dve", identb[:], identf[:], [r_idf], [r_idb])
                        rin, r_rin = sc.sb([128, 12])
                        riny, r_riny = sc.sb([128, 4])
                        ows = [sc.sb([128, 260]) for _ in range(2)]
                        nps = [0]

                        def score_exp(lhsT, rl, rhs, rr_, np_, extra=None):
                            ps_, rps = pS[nps[0] % 3]
                            pt_, rpt = pT[nps[0] % 3]
                            if not Sc.mute:
                                nps[0] += 1
                            Sc.mm(ps_[0:np_, :], lhsT, rhs, True, extra is None, rl + rr_, [rps])
                            if extra is not None:
                                l2, r2, rl2 = extra
                                Sc.mm(ps_[0:np_, :], l2, r2, False, True, rl2, [rps])
                            Sc.act(pt_[0:np_, :], ps_[0:np_, :], AF.Exp, [rps], [rpt], scale=0.125)
                            return pt_, rpt

                        def qtile(qt, part):
                            q_a, r_qa = qa[qt % 2]
                            g_t, r_gt = gt[qt % 2]
                            m_t, r_mt = mt[qt % 2]
                            a_t, r_at = at[qt % 2]
                            o_a, r_oa = oacc[qt % 2]
                            q0 = qt * 128
                            ow, r_ow = ows[qt % 2]
                            Sc.mute = part != "X"
                            Sc.dma("sp", q_a[0:64, :].rearrange("p (g t) -> p g t", g=4),
                                   fmT.ap()[hk * 256:(hk + 1) * 256, q0:q0 + 128].rearrange("(g p) t -> p g t", p=64),
                                   writes=[r_qa])
                            Sc.dma(dq(), g_t[:], gates_d.ap()[q0:q0 + 128, :], writes=[r_gt])
                            Sc.dma(dq(), m_t[:], cd["mtk"].ap()[qt], writes=[r_mt])
                            Sc.dma(dq(), a_t[:], cd["atk"].ap()[qt], writes=[r_at])
                            nvalid = min(NCMP, 8 * qt + 8)
                            for pr in range(2):
                                Sc.memset("dve", pc[pr][0][:, 0:258], 0.0, [pc[pr][1]])
                            Sc.memset("dve", po_w[:, 0:260], 0.0, [rpo_w])

                            def st1(item):
                                kind, j = item
                                if kind == "c":
                                    M = min(128, nvalid - j * 128)
                                    zoff = 256 + 128 * j - 8 * qt + 8
                                    return score_exp(kc[:, j * 128:j * 128 + M], [r_kc], q_a[0:64, :], [r_qa], M,
                                                     extra=(zz[:, zoff:zoff + M], bc[:], [r_zz, r_bc]))
                                if kind == "w":
                                    kt = qt - j
                                    ptw, rptw = score_exp(kwT[:, kt * 128:(kt + 1) * 128], [r_kwT], q_a[0:64, :], [r_qa], 128)
                                    if j == 0:
                                        Sc.tt("pool", ptw[:], ptw[:], eb0[:], ALU.mult, [rptw, r_eb0], [rptw])
                                    elif j == 1:
                                        Sc.tt("pool", ptw[:], ptw[:], eb1[:], ALU.mult, [rptw, r_eb1], [rptw])
                                    elif j == 4:
                                        Sc.tt("pool", ptw[:], ptw[:], wm[:], ALU.mult, [rptw, r_wm], [rptw])
                                    return ptw, rptw
                                kt = j
                                pts, rpts = score_exp(ksT[:, kt * 128:(kt + 1) * 128], [r_ksT], q_a[:], [r_qa], 128)
                                if qt - kt == 0:
                                    Sc.tt("pool", pts[:], pts[:], eb0[:], ALU.mult, [rpts, r_eb0], [rpts])
                                elif qt - kt == 1:
                                    Sc.tt("pool", pts[:], pts[:], eb1[:], ALU.mult, [rpts, r_eb1], [rpts])
                                return pts, rpts

                            def st2(item, res_):
                                kind, j = item
                                pt_, rpt_ = res_
                                if kind == "c":
                                    M = min(128, nvalid - j * 128)
                                    for g in range(4):
                                        pcc, rpcc = pc[g // 2]
                                        Sc.mm(pcc[:, (g % 2) * 129:(g % 2) * 129 + 129], pt_[0:M, g * 128:(g + 1) * 128],
                                              vce[0:M, j, :], False, False, [rpt_, r_vce], [rpcc], skip=True)
                                elif kind == "w":
                                    kt = qt - j
                                    for g in range(4):
                                        Sc.mm(po_w[:, g * 65:g * 65 + 65], pt_[:, g * 128:(g + 1) * 128], vw[:, kt, :],
                                              False, False, [rpt_, r_vw], [rpo_w], skip=True)
                                else:
                                    for g in range(4):
                                        Sc.mm(po_s[:, g * 65:g * 65 + 65], pt_[:, g * 128:(g + 1) * 128], vs[:, j, :],
                                              False, False, [rpt_, r_vs], [rpo_s], skip=True)

                            def run_pipe(items, depth=2):
                                pend = []
                                for it in items:
                                    pend.append((it, st1(it)))
                                    if len(pend) > depth:
                                        st2(*pend.pop(0))
                                while pend:
                                    st2(*pend.pop(0))

                            c_items = [("c", ct) for ct in range(NCT) if min(128, nvalid - ct * 128) > 0]
                            w_items = [("w", dl) for dl in range(5) if qt - dl >= 0]
                            s_items = [("s", kt) for kt in range(qt + 1)]
                            run_pipe(c_items + w_items)
                            for g in range(4):
                                pcc, rpcc = pc[g // 2]
                                b0 = (g % 2) * 129
                                Sc.ts("dve", rin[:, g:g + 1], pcc[:, b0 + 64:b0 + 65], 1e-30, None, ALU.max, None,
                                      [rpcc], [r_rin])
                            Sc.op("dve", lambda e: e.reciprocal(out=rin[:, 0:4], in_=rin[:, 0:4]), [r_rin], [r_rin])
                            for g in range(4):
                                pcc, rpcc = pc[g // 2]
                                b0 = (g % 2) * 129
                                if g == 0:
                                    Sc.ts("dve", imp[:], pcc[:, b0 + 65:b0 + 129], rin[:, 0:1], None, ALU.mult, None,
                                          [rpcc, r_rin], [r_imp])
                                else:
                                    Sc.stt(imp[:], pcc[:, b0 + 65:b0 + 129], rin[:, g:g + 1], imp[:], ALU.mult, ALU.add,
                                           [rpcc, r_rin, r_imp], [r_imp])
                            Sc.tt("dve", rin[:, 4:8], rin[:, 0:4], g_t[:, hk * 12:hk * 12 + 12:3], ALU.mult, [r_rin, r_gt], [r_rin])
                            for g in range(4):
                                pcc, rpcc = pc[g // 2]
                                b0 = (g % 2) * 129
                                Sc.ts("dve", o_a[:, g, :], pcc[:, b0:b0 + 64], rin[:, 4 + g:5 + g], None, ALU.mult, None,
                                      [rpcc, r_rin], [r_oa])
                            Sc.tt("dve", imod[:], imp[:], m_t[:], ALU.mult, [r_imp, r_mt], [r_imod])
                            Sc.tt("dve", imod[:], imod[:], a_t[:], ALU.add, [r_imod, r_at], [r_imod])
                            Sc.op("dve", lambda e: e.max(out=m8[:], in_=imod[:]), [r_imod], [r_m8])
                            Sc.op("dve", lambda e: e.match_replace(out=w1_[:], in_to_replace=m8[:], in_values=imod[:],
                                                                   imm_value=-3e38), [r_m8, r_imod], [r_w1_])
                            Sc.op("dve", lambda e: e.max(out=m8[:], in_=w1_[:]), [r_w1_], [r_m8])
                            Sc.op("dve", lambda e: e.match_replace(out=w2_[:], in_to_replace=m8[:], in_values=w1_[:],
                                                                   imm_value=-3e38), [r_m8, r_w1_], [r_w2_])
                            Sc.tt("dve", w1_[:], w2_[:], imod[:], ALU.not_equal, [r_w2_, r_imod], [r_w1_])
                            Sc.ts("dve", selb[:, 64:128], w1_[:], -1.0, -NEGB, ALU.add, ALU.mult, [r_w1_], [r_selb])
                            Sc.tr(ptT[:, 0:128], selb[:], identb[:], [r_selb, r_idb], [rptT])
                            for g in range(4):
                                Sc.cp("act", q_a[64:128, g * 128:(g + 1) * 128], ptT[64:128, 0:128],
                                      [rptT], [r_qa])
                            Sc.cp("dve", ow[:], po_w[:, 0:260], [rpo_w], [r_ow])
                            Sc.mute = part != "Y"
                            Sc.memset("dve", po_s[:, 0:260], 0.0, [rpo_s])
                            run_pipe(s_items)

                            for bi, (pp, rpp) in enumerate(((po_s, rpo_s), (ow, r_ow))):
                                if bi == 1 and qt < 4:
                                    c8 = qt * 8 + hk * 4
                                    Sc.tt("dve", riny[:, 0:4], pp[:, 64:260:65], xdn[:, c8:c8 + 4], ALU.add,
                                          [rpp, r_xdn], [r_riny])
                                    Sc.op("dve", lambda e: e.reciprocal(out=riny[:, 0:4], in_=riny[:, 0:4]),
                                          [r_riny], [r_riny])
                                else:
                                    Sc.op("dve", lambda e, pp=pp: e.reciprocal(out=riny[:, 0:4], in_=pp[:, 64:260:65]),
                                          [rpp], [r_riny])
                                Sc.tt("dve", riny[:, 0:4], riny[:, 0:4], g_t[:, hk * 12 + 1 + bi:hk * 12 + 12:3], ALU.mult, [r_riny, r_gt],
                                      [r_riny])
                                for g in range(4):
                                    Sc.stt(o_a[:, g, :], pp[:, g * 65:g * 65 + 64], riny[:, g:g + 1], o_a[:, g, :],
                                           ALU.mult, ALU.add, [rpp, r_riny, r_oa], [r_oa])
                            o_b, r_ob = obf[qt % 2]
                            Sc.cp("act", o_b[:], o_a[:].rearrange("p g d -> p (g d)"), [r_oa], [r_ob])
                            Sc.dma("sp", o_all.ap()[q0:q0 + 128, 512 + hk * 256:512 + (hk + 1) * 256], o_b[:], reads=[r_ob])
                            Sc.mute = False

                        qtile(0, "X")
                        for qt in range(NT):
                            if qt + 1 < NT:
                                qtile(qt + 1, "X")
                            qtile(qt, "Y")
                if stop_after == "C%d" % l:
                    return nc

                with Scope() as sc:
                    tris, r_tris = sc.sb([128, 128])
                    trib, r_trib = sc.sb([128, 128], BF16)
                    Sc.dma("sp", tris[:], cd["tri"].ap(), writes=[r_tris])
                    Sc.cp("dve", trib[:], tris[:], [r_tris], [r_trib])
                    kT = [sc.sb([96, S], BF16) for _ in range(2)]
                    va = [sc.sb([128, NT, 65], BF16) for _ in range(2)]
                    for i in range(2):
                        Sc.memset("dve", va[i][0][:], 1.0, [va[i][1]])
                    qT = [sc.sb([96, 512], BF16) for _ in range(2)]
                    NBUF_D = 5
                    pS = [sc.ps() for _ in range(NBUF_D)]
                    pT = [sc.sb([128, 512], BF16) for _ in range(NBUF_D)]
                    pO = [sc.ps() for _ in range(2)]
                    rin = [sc.sb([128, 4]) for _ in range(2)]
                    ob = [sc.sb([128, 4, 64], BF16) for _ in range(2)]
                    nps_box = [0]
                    nq = 0
                    for h in range(8):
                        k_T, r_kT = kT[h % 2]
                        v_a, r_va = va[h % 2]
                        Sc.dma("sp", k_T[0:64, :], knT.ap()[h * 64:(h + 1) * 64, :], writes=[r_kT])
                        Sc.dma(dq(), k_T[64:96, :], krT.ap(), writes=[r_kT])
                        dma_split("sp", v_a[:, :, 0:64],
                                  vmla.ap()[:, h * 64:(h + 1) * 64].rearrange("(t p) d -> p t d", p=128), 1, 4,
                                  writes=[r_va])
                        for qb in range(NB):
                            q_T, r_qT = qT[nq % 2]
                            p_O, rpO = pO[nq % 2]
                            r_i, r_ri = rin[nq % 2]
                            o_b, r_ob = ob[nq % 2]
                            nq += 1
                            c0 = qb * 512
                            Sc.dma("sp", q_T[0:64, :], qnT.ap()[h * 64:(h + 1) * 64, c0:c0 + 512], writes=[r_qT])
                            Sc.dma(dq(), q_T[64:96, :], qrT.ap()[h * 32:(h + 1) * 32, c0:c0 + 512], writes=[r_qT])
                            Sc.memset("dve", p_O[:, 0:260], 0.0, [rpO])
                            def d1(kt):
                                nonlocal_nps = nps_box[0]
                                nps_box[0] += 1
                                j = kt - 4 * qb
                                qs = max(0, j) * 128
                                ps_, rps = pS[nonlocal_nps % NBUF_D]
                                pt_, rpt = pT[nonlocal_nps % NBUF_D]
                                Sc.mm(ps_[:, qs:512], k_T[:, kt * 128:(kt + 1) * 128], q_T[:, qs:512], True, True,
                                      [r_kT, r_qT], [rps])
                                Sc.act(pt_[:, qs:512], ps_[:, qs:512], AF.Exp, [rps], [rpt], scale=96 ** -0.5)
                                if j >= 0:
                                    Sc.tt("pool", pt_[:, qs:qs + 128], pt_[:, qs:qs + 128], trib[:], ALU.mult,
                                          [rpt, r_trib], [rpt])
                                return pt_, rpt

                            def d2(kt, res_):
                                pt_, rpt = res_
                                j = kt - 4 * qb
                                for qi in range(max(0, j), 4):
                                    Sc.mm(p_O[:, qi * 65:qi * 65 + 65], pt_[:, qi * 128:(qi + 1) * 128], v_a[:, kt, :],
                                          False, False, [rpt, r_va], [rpO], skip=True)

                            pend = []
                            for kt in range(4 * qb + 4):
                                pend.append((kt, d1(kt)))
                                if len(pend) > NBUF_D - 2:
                                    d2(*pend.pop(0))
                            while pend:
                                d2(*pend.pop(0))

                            Sc.op("dve", lambda e, p_O=p_O, r_i=r_i: e.reciprocal(out=r_i[:], in_=p_O[:, 64:260:65]),
                                  [rpO], [r_ri])
                            for qi in range(4):
                                Sc.ts("dve", o_b[:, qi, :], p_O[:, qi * 65:qi * 65 + 64], r_i[:, qi:qi + 1], None,
                                      ALU.mult, None, [rpO, r_ri], [r_ob])
                            Sc.dma("sp", o_all.ap()[c0:c0 + 512, h * 64:(h + 1) * 64].rearrange("(q p) d -> p q d", p=128),
                                   o_b[:], reads=[r_ob])
                if stop_after == "D%d" % l:
                    return nc

                with Scope() as sc:
                    identf, r_idf = sc.sb([128, 128])
                    identb, r_idb = sc.sb([128, 128], BF16)
                    Sc.dma("sp", identf[:], cd["identf"].ap(), writes=[r_idf])
                    Sc.cp("dve", identb[:], identf[:], [r_idf], [r_idb])
                    gpost, r_gpost = sc.sb([128, 1024])
                    Sc.dma("sp", gpost[:], bcast_rows(Wl["g_post"], 0, 128, 1024), writes=[r_gpost])
                    wo, r_wo = sc.sb([128, 8, 1024], BF16)
                    load_cast(sc, wo[:], r_wo, Wl["w_o"].ap().rearrange("(k p) n -> p k n", p=128), [128, 8, 1024])
                    ot = [sc.sb([128, 1024], BF16) for _ in range(2)]
                    xt = [sc.sb([128, 1024]) for _ in range(2)]
                    ptr = [sc.ps([128, 1024], BF16) for _ in range(2)]
                    oT = [sc.sb([128, 8, 128], BF16) for _ in range(2)]
                    py = [sc.ps() for _ in range(4)]
                    junk, r_junk = sc.sb([128, 1024], BF16)
                    ysb = [sc.sb([128, 1024]) for _ in range(2)]
                    xo = [sc.sb([128, 1024]) for _ in range(2)]
                    def front(ti):
                        o_t, r_ot = ot[ti % 2]
                        x_t, r_x = xt[ti % 2]
                        p_t, r_pt = ptr[ti % 2]
                        o_T, r_oT = oT[ti % 2]
                        rows = slice(ti * 128, (ti + 1) * 128)
                        Sc.dma("sp", o_t[:], o_all.ap()[rows, :], writes=[r_ot])
                        Sc.dma(dq(), x_t[:], x_src[rows, :], writes=[r_x])
                        for k in range(8):
                            Sc.tr(p_t[:, k * 128:(k + 1) * 128], o_t[:, k * 128:(k + 1) * 128], identb[:],
                                  [r_ot, r_idb], [r_pt])
                        Sc.cp("act", o_T[:], p_t[:].rearrange("p (k t) -> p k t", k=8), [r_pt], [r_oT])
                        for hf in range(2):
                            p_y, rpy = py[(ti % 2) * 2 + hf]
                            for k in range(8):
                                Sc.mm(p_y[:], o_T[:, k, :], wo[:, k, hf * 512:(hf + 1) * 512], k == 0, k == 7,
                                      [r_oT, r_wo], [rpy])

                    def back(ti):
                        x_t, r_x = xt[ti % 2]
                        y_s, r_ys = ysb[ti % 2]
                        x_o, r_xo = xo[ti % 2]
                        rows = slice(ti * 128, (ti + 1) * 128)
                        for hf in range(2):
                            p_y, rpy = py[(ti % 2) * 2 + hf]
                            Sc.cp("act" if hf else "dve", y_s[:, hf * 512:(hf + 1) * 512], p_y[:], [rpy], [r_ys])
                        ss, rss = rms_rstd(sc, y_s[:], 1024, junk[:], r_junk, [r_ys])
                        Sc.stt(y_s[:], y_s[:], ss[:, 0:1], gpost[:], ALU.mult, ALU.mult, [r_ys, rss, r_gpost], [r_ys])
                        Sc.tt("pool", x_o[:], y_s[:], x_t[:], ALU.add, [r_ys, r_x], [r_xo])
                        Sc.dma("sp", xs1.ap()[rows, :], x_o[:], reads=[r_xo])

                    front(0)
                    for ti in range(NT):
                        if ti + 1 < NT:
                            front(ti + 1)
                        back(ti)
                if stop_after == "E%d" % l:
                    return nc

                with Scope() as sc:
                    identf, r_idf = sc.sb([128, 128])
                    identb, r_idb = sc.sb([128, 128], BF16)
                    Sc.dma("sp", identf[:], cd["identf"].ap(), writes=[r_idf])
                    Sc.cp("dve", identb[:], identf[:], [r_idf], [r_idb])
                    gf, r_gf = sc.sb([128, 1024])
                    Sc.dma("sp", gf[:], bcast_rows(Wl["g_fpre"], 0, 128, 1024), writes=[r_gf])
                    wg, r_wg = sc.sb([128, 8, 2816], BF16)
                    wu, r_wu = sc.sb([128, 8, 2816], BF16)
                    stg = [sc.sb([128, 8, 352]) for _ in range(2)]
                    for wi, (wt_, rw_, src) in enumerate(((wg, r_wg, Wl["w_gate"]), (wu, r_wu, Wl["w_up"]))):
                        v = src.ap().rearrange("(k p) n -> p k n", p=128)
                        for cc in range(8):
                            load_cast(sc, wt_[:, :, cc * 352:(cc + 1) * 352], rw_, v[:, :, cc * 352:(cc + 1) * 352], None,
                                      stage=stg[(wi * 8 + cc) % 2])
                    cw, r_cw = sc.sb([128, 66])
                    cb, r_cb = sc.sb([128, 22])
                    Sc.dma("sp", cw[:], Wl["conv_w"].ap(), writes=[r_cw])
                    Sc.dma("sp", cb[:], Wl["conv_b"].ap(), writes=[r_cb])
                    gprev, r_gprev = sc.sb([128, 22, 2])
                    Sc.memset("dve", gprev[:], 0.0, [r_gprev])
                    xt = [sc.sb([128, 1024]) for _ in range(2)]
                    junk, r_junk = sc.sb([128, 1024], BF16)
                    hb = [sc.sb([128, 1024], BF16) for _ in range(2)]
                    ptr = [sc.ps([128, 1024], BF16) for _ in range(2)]
                    hT = [sc.sb([128, 8, 512], BF16) for _ in range(2)]
                    pg = [sc.ps() for _ in range(2)]
                    pu = [sc.ps() for _ in range(2)]
                    gsh = [sc.sb([128, 514]) for _ in range(2)]
                    gc = [sc.sb([128, 512]) for _ in range(2)]
                    ao = [sc.sb([128, 512], BF16) for _ in range(2)]
                    n = 0
                    for tb_ in range(NB):
                        c0 = tb_ * 512
                        hTt, r_hT = hT[tb_ % 2]
                        for i in range(4):
                            ti = tb_ * 4 + i
                            (x_t, r_x) = xt[ti % 2]
                            (h_b, r_hb) = hb[ti % 2]
                            (p_t, r_pt) = ptr[ti % 2]
                            Sc.dma(dq(), x_t[:], xs1.ap()[ti * 128:(ti + 1) * 128, :], writes=[r_x])
                            ss, rss = rms_rstd(sc, x_t[:], 1024, junk[:], r_junk, [r_x])
                            Sc.stt(h_b[:], x_t[:], ss[:, 0:1], gf[:], ALU.mult, ALU.mult, [r_x, rss, r_gf], [r_hb])
                            for k in range(8):
                                Sc.tr(p_t[:, k * 128:(k + 1) * 128], h_b[:, k * 128:(k + 1) * 128], identb[:],
                                      [r_hb, r_idb], [r_pt])
                            Sc.cp("act", hTt[:, :, i * 128:(i + 1) * 128],
                                  p_t[:].rearrange("p (k t) -> p k t", k=8), [r_pt], [r_hT])
                        for fc in range(22):
                            p_g, rpg = pg[n % 2]
                            p_u, rpu = pu[n % 2]
                            g_s, r_gs = gsh[n % 2]
                            g_c, r_gc = gc[n % 2]
                            a_o, r_ao = ao[n % 2]
                            n += 1
                            for k in range(8):
                                Sc.mm(p_g[:], wg[:, k, fc * 128:(fc + 1) * 128], hTt[:, k, :], k == 0, k == 7,
                                      [r_wg, r_hT], [rpg])
                            for k in range(8):
                                Sc.mm(p_u[:], wu[:, k, fc * 128:(fc + 1) * 128], hTt[:, k, :], k == 0, k == 7,
                                      [r_wu, r_hT], [rpu])
                            Sc.cp("pool", g_s[:, 0:2], gprev[:, fc, :], [r_gprev], [r_gs])
                            Sc.cp("act", g_s[:, 2:514], p_g[:], [rpg], [r_gs])
                            Sc.cp("pool", gprev[:, fc, :], g_s[:, 512:514], [r_gs], [r_gprev])
                            Sc.ts("dve", g_c[:], g_s[:, 2:514], cw[:, fc * 3 + 2:fc * 3 + 3], cb[:, fc:fc + 1], ALU.mult,
                                  ALU.add, [r_gs, r_cw, r_cb], [r_gc])
                            Sc.stt(g_c[:], g_s[:, 1:513], cw[:, fc * 3 + 1:fc * 3 + 2], g_c[:], ALU.mult, ALU.add,
                                   [r_gs, r_cw, r_gc], [r_gc])
                            Sc.stt(g_c[:], g_s[:, 0:512], cw[:, fc * 3:fc * 3 + 1], g_c[:], ALU.mult, ALU.add,
                                   [r_gs, r_cw, r_gc], [r_gc])
                            Sc.act(g_c[:], g_c[:], AF.Gelu_apprx_tanh, [r_gc], [r_gc])
                            Sc.tt("dve", a_o[:], g_c[:], p_u[:], ALU.mult, [r_gc, rpu], [r_ao])
                            Sc.dma(dq(), aT_d.ap()[fc * 128:(fc + 1) * 128, c0:c0 + 512], a_o[:], reads=[r_ao])
                if stop_after == "F1%d" % l:
                    return nc

                with Scope() as sc:
                    gp, r_gp = sc.sb([128, 1024])
                    Sc.dma("sp", gp[:], bcast_rows(Wl["g_fpost"], 0, 128, 1024), writes=[r_gp])
                    wd, r_wd = sc.sb([128, 22, 1024], BF16)
                    stg = [sc.sb([128, 2, 1024]) for _ in range(2)]
                    v = Wl["w_down"].ap().rearrange("(k p) n -> p k n", p=128)
                    for cc in range(11):
                        load_cast(sc, wd[:, cc * 2:cc * 2 + 2, :], r_wd, v[:, cc * 2:cc * 2 + 2, :], None, stage=stg[cc % 2])
                    aT = [sc.sb([128, 22, 128], BF16) for _ in range(2)]
                    xt = [sc.sb([128, 1024]) for _ in range(2)]
                    py = [sc.ps() for _ in range(4)]
                    junk, r_junk = sc.sb([128, 1024], BF16)
                    ysb = [sc.sb([128, 1024]) for _ in range(2)]
                    xo = [sc.sb([128, 1024]) for _ in range(2)]
                    def front(ti):
                        a_T, r_aT = aT[ti % 2]
                        x_t, r_x = xt[ti % 2]
                        rows = slice(ti * 128, (ti + 1) * 128)
                        dma_split("sp", a_T[:], aT_d.ap()[:, ti * 128:(ti + 1) * 128].rearrange("(k p) t -> p k t", p=128),
                                  1, 4, writes=[r_aT])
                        Sc.dma(dq(), x_t[:], xs1.ap()[rows, :], writes=[r_x])
                        for hf in range(2):
                            p_y, rpy = py[(ti % 2) * 2 + hf]
                            for k in range(22):
                                Sc.mm(p_y[:], a_T[:, k, :], wd[:, k, hf * 512:(hf + 1) * 512], k == 0, k == 21,
                                      [r_aT, r_wd], [rpy])

                    def back(ti):
                        x_t, r_x = xt[ti % 2]
                        y_s, r_ys = ysb[ti % 2]
                        x_o, r_xo = xo[ti % 2]
                        rows = slice(ti * 128, (ti + 1) * 128)
                        for hf in range(2):
                            p_y, rpy = py[(ti % 2) * 2 + hf]
                            Sc.cp("act" if hf else "dve", y_s[:, hf * 512:(hf + 1) * 512], p_y[:], [rpy], [r_ys])
                        ss, rss = rms_rstd(sc, y_s[:], 1024, junk[:], r_junk, [r_ys])
                        Sc.stt(y_s[:], y_s[:], ss[:, 0:1], gp[:], ALU.mult, ALU.mult, [r_ys, rss, r_gp], [r_ys])
                        Sc.tt("pool", x_o[:], y_s[:], x_t[:], ALU.add, [r_ys, r_x], [r_xo])
                        Sc.dma("sp", xs2.ap()[rows, :], x_o[:], reads=[r_xo])

                    front(0)
                    for ti in range(NT):
                        if ti + 1 < NT:
                            front(ti + 1)
                        back(ti)
                if stop_after == "F2%d" % l:
                    return nc

                with Scope() as sc:
                    identf, r_idf = sc.sb([128, 128])
                    identb, r_idb = sc.sb([128, 128], BF16)
                    Sc.dma("sp", identf[:], cd["identf"].ap(), writes=[r_idf])
                    Sc.cp("dve", identb[:], identf[:], [r_idf], [r_idb])
                    wpg, r_wpg = sc.sb([128, 8, 1024], BF16)
                    load_cast(sc, wpg[:], r_wpg, Wl["ple_gate"].ap().rearrange("(k p) n -> p k n", p=128), [128, 8, 1024])
                    wpp, r_wpp = sc.sb([128, 2, 1024], BF16)
                    load_cast(sc, wpp[:], r_wpp, Wl["ple_proj"].ap().rearrange("(k p) n -> p k n", p=128), [128, 2, 1024])
                    xt = [sc.sb([128, 1024]) for _ in range(2)]
                    pt_in = [sc.sb([128, 256]) for _ in range(2)]
                    xb = [sc.sb([128, 1280], BF16) for _ in range(2)]
                    ptr = [sc.ps([128, 1024], BF16) for _ in range(2)]
                    ptp = sc.ps([128, 1024], BF16)
                    xT = [sc.sb([128, 10, 128], BF16) for _ in range(2)]
                    pgt = [sc.ps() for _ in range(2)]
                    pe_ = [sc.ps() for _ in range(2)]
                    sg = [sc.sb([128, 1024]) for _ in range(2)]
                    xo = [sc.sb([128, 1024]) for _ in range(2)]
                    def front(ti):
                        x_t, r_x = xt[ti % 2]
                        p_i, r_pi = pt_in[ti % 2]
                        x_b, r_xb = xb[ti % 2]
                        p_t, r_pt = ptr[ti % 2]
                        x_T, r_xT = xT[ti % 2]
                        rows = slice(ti * 128, (ti + 1) * 128)
                        Sc.dma("sp", x_t[:], xs2.ap()[rows, :], writes=[r_x])
                        Sc.dma(dq(), p_i[:], p_in.ap()[l, bi_, rows, :], writes=[r_pi])
                        Sc.cp("dve", x_b[:, 0:1024], x_t[:], [r_x], [r_xb])
                        Sc.cp("pool", x_b[:, 1024:1280], p_i[:], [r_pi], [r_xb])
                        for k in range(8):
                            Sc.tr(p_t[:, k * 128:(k + 1) * 128], x_b[:, k * 128:(k + 1) * 128], identb[:],
                                  [r_xb, r_idb], [r_pt])
                        for k in range(2):
                            Sc.tr(ptp[0][:, k * 128:(k + 1) * 128], x_b[:, 1024 + k * 128:1024 + (k + 1) * 128], identb[:],
                                  [r_xb, r_idb], [ptp[1]])
                        Sc.cp("act", x_T[:, 0:8, :], p_t[:].rearrange("p (k t) -> p k t", k=8), [r_pt], [r_xT])
                        Sc.cp("act", x_T[:, 8:10, :], ptp[0][:, 0:256].rearrange("p (k t) -> p k t", k=2), [ptp[1]],
                              [r_xT])

                    def back(ti):
                        x_t, r_x = xt[ti % 2]
                        x_T, r_xT = xT[ti % 2]
                        s_g, r_sg = sg[ti % 2]
                        x_o, r_xo = xo[ti % 2]
                        rows = slice(ti * 128, (ti + 1) * 128)
                        for hf in range(2):
                            p_g, rpg = pgt[hf]
                            p_e, rpe = pe_[hf]
                            for k in range(8):
                                Sc.mm(p_g[:], x_T[:, k, :], wpg[:, k, hf * 512:(hf + 1) * 512], k == 0, k == 7,
                                      [r_xT, r_wpg], [rpg])
                            for k in range(2):
                                Sc.mm(p_e[:], x_T[:, 8 + k, :], wpp[:, k, hf * 512:(hf + 1) * 512], k == 0, k == 1,
                                      [r_xT, r_wpp], [rpe])
                            Sc.act(s_g[:, hf * 512:(hf + 1) * 512], p_g[:], AF.Sigmoid, [rpg], [r_sg])
                            Sc.tt("dve", s_g[:, hf * 512:(hf + 1) * 512], s_g[:, hf * 512:(hf + 1) * 512], p_e[:], ALU.mult,
                                  [r_sg, rpe], [r_sg])
                        Sc.tt("pool", x_o[:], s_g[:], x_t[:], ALU.add, [r_sg, r_x], [r_xo])
                        Sc.dma("sp", x_dst[rows, :], x_o[:], reads=[r_xo])

                    front(0)
                    for ti in range(NT):
                        if ti + 1 < NT:
                            front(ti + 1)
                        back(ti)
                x_src = xs3.ap()
    return nc


def prep_weights(l, w_in, mla_q_norm, mla_w_uq, mla_kv_norm, mla_w_ukv, nsa_cmp_pos, nsa_cmp_w1, nsa_cmp_w2,
                 w_o, ffn_w_gate, ffn_w_up, ffn_conv_w, ffn_conv_b, ffn_w_down, ple_proj, ple_gate,
                 attn_pre_norm, attn_post_norm, ffn_pre_norm, ffn_post_norm):
    f = lambda a: np.ascontiguousarray(np.asarray(a, dtype=np.float32))
    wi = np.asarray(w_in[l], dtype=np.float32)
    offs = np.cumsum([0, 256, 128, 32, 512, 128, 128, 128, 128, 128, 128, 24])
    seg = lambda i: wi[:, offs[i]:offs[i + 1]]
    c_q, c_kv, k_rope, q_nsa, k_cmp, v_cmp, k_slc, v_slc, k_win, v_win, g_nsa = [seg(i) for i in range(11)]
    d = {}
    d["w_fm"] = f(np.concatenate([q_nsa, k_cmp, v_cmp, k_slc, k_win], axis=1))
    d["w_kr"] = f(np.concatenate([k_rope, k_rope[:, 16:32], k_rope[:, 0:16]], axis=1))
    d["w_tm"] = f(np.concatenate([c_q, c_kv, v_slc, v_win, g_nsa], axis=1))
    uq = np.asarray(mla_w_uq[l], dtype=np.float32).reshape(256, 8, 96)
    d["wuq_n"] = f(uq[:, :, 0:64].reshape(256, 512))
    ra = uq[:, :, 64:96]
    rb = np.concatenate([uq[:, :, 80:96], uq[:, :, 64:80]], axis=2)
    d["wuq_r"] = f(np.concatenate([ra.reshape(256, 256), rb.reshape(256, 256)], axis=1))
    ukv = np.asarray(mla_w_ukv[l], dtype=np.float32).reshape(128, 8, 128)
    d["wukv_k"] = f(ukv[:, :, 0:64].reshape(128, 512))
    d["wukv_v"] = f(ukv[:, :, 64:128].reshape(128, 512))
    d["qn_g"] = f(np.asarray(mla_q_norm[l]).reshape(1, 256))
    d["kvn_g"] = f(np.asarray(mla_kv_norm[l]).reshape(1, 128))
    d["cmp_pos"] = f(np.asarray(nsa_cmp_pos[l]).transpose(0, 2, 1))
    w1 = np.asarray(nsa_cmp_w1[l], dtype=np.float32).reshape(2, 32, 64, 128)
    d["cmp_w1"] = f(w1.transpose(0, 2, 1, 3).reshape(2, 64, 32 * 128))
    d["cmp_w2"] = f(nsa_cmp_w2[l])
    d["w_o"] = f(w_o[l])
    d["w_gate"] = f(ffn_w_gate[l])
    d["w_up"] = f(ffn_w_up[l])
    cw = np.asarray(ffn_conv_w[l], dtype=np.float32)
    d["conv_w"] = f(cw.T.reshape(22, 128, 3).transpose(1, 0, 2).reshape(128, 66))
    d["conv_b"] = f(np.asarray(ffn_conv_b[l], dtype=np.float32).reshape(22, 128).T)
    d["w_down"] = f(ffn_w_down[l])
    d["ple_proj"] = f(ple_proj[l])
    d["ple_gate"] = f(ple_gate[l])
    d["g_pre"] = f(np.asarray(attn_pre_norm[l]).reshape(1, 1024))
    d["g_post"] = f(np.asarray(attn_post_norm[l]).reshape(1, 1024))
    d["g_fpre"] = f(np.asarray(ffn_pre_norm[l]).reshape(1, 1024))
    d["g_fpost"] = f(np.asarray(ffn_post_norm[l]).reshape(1, 1024))
    return d


def make_in_maps(x, p, positions, rel_bias, L, ncores, nbc=1, **w):
    B, S, _ = x.shape
    consts = make_consts(S)
    shared = {"relb": np.ascontiguousarray(np.asarray(rel_bias, dtype=np.float32))}
    for k, v in consts.items():
        shared["c_" + k] = np.ascontiguousarray(v, dtype=np.float32)
    for l in range(L):
        for k, v in prep_weights(l, **w).items():
            shared["l%d_%s" % (l, k)] = v
    maps = []
    for c in range(ncores):
        b0 = (c * nbc) % B
        m = dict(shared)
        m["x"] = np.ascontiguousarray(np.asarray(x[b0:b0 + nbc], dtype=np.float32))
        m["p"] = np.ascontiguousarray(np.asarray(p[:, b0:b0 + nbc], dtype=np.float32))
        m["pos"] = np.ascontiguousarray(np.asarray(positions[b0:b0 + nbc], dtype=np.int32).reshape(nbc, S))
        maps.append(m)
    return maps


N_CORES = 4


def kernel(x, p, positions, rel_bias, **w):
    x = np.asarray(x)
    B, S, D = x.shape
    L = np.asarray(p).shape[0]
    nbc = B // N_CORES
    nc = build(S=S, L=L, NBC=nbc)
    maps = make_in_maps(x, np.asarray(p), np.asarray(positions), rel_bias, L, N_CORES, nbc=nbc, **w)
    res = run_bass_kernel_spmd(nc, maps, core_ids=list(range(N_CORES)))
    out = np.concatenate([np.asarray(res.results[c]["out"], dtype=np.float32) for c in range(N_CORES)], axis=0)
    return out.reshape(B, S, D)
```

```python
import contextlib
import math
import numpy as np
import concourse.bass as bass
import concourse.mybir as mybir
from concourse.bass_utils import run_bass_kernel_spmd

F32 = mybir.dt.float32
BF16 = mybir.dt.bfloat16
I32 = mybir.dt.int32
AF = mybir.ActivationFunctionType
ALU = mybir.AluOpType

N_DMA_SEMS = 8
USE_POOL_DMA = False
SAME_ENG_SYNC = True
EPS = 1e-6
NEGB = -30000.0


class Res:
    __slots__ = ("lw", "rd_eng", "rd_dma", "pg", "pre")

    def __init__(self):
        self.lw = []
        self.rd_eng = {}
        self.rd_dma = []
        self.pg = None
        self.pre = []


class Op:
    __slots__ = ("eng", "fn", "dma", "deps", "signal", "count", "sem", "val")

    def __init__(self, eng, fn, dma):
        self.eng = eng
        self.fn = fn
        self.dma = dma
        self.deps = ()
        self.signal = False
        self.count = 0
        self.sem = None
        self.val = 0


class Sched:
    ENGS = ("pe", "act", "dve", "pool", "sp")
    OBJ = {"pe": "tensor", "act": "scalar", "dve": "vector", "pool": "gpsimd", "sp": "sync"}

    def __init__(self, nc, st):
        self.nc = nc
        self.q = {e: [] for e in self.ENGS}
        self.esem = {e: st.enter_context(nc.semaphore("c_" + e)) for e in self.ENGS if e != "sp"}
        self.dsem = {e: [st.enter_context(nc.semaphore("d_%s%d" % (e, i))) for i in range(N_DMA_SEMS)]
                     for e in ("sp", "pool")}
        self.ecount = {e: 0 for e in self.ENGS}
        self.dk = {e: 0 for e in ("sp", "pool")}
        self.nops = 0

    mute = False

    def op(self, eng, fn, reads=(), writes=(), dma=False, par=None):
        if self.mute:
            return None
        o = Op(eng, fn, dma)
        self.nops += 1
        deps = {}
        for r in reads:
            for d in r.lw:
                deps[id(d)] = d
        for w in writes:
            if par is not None and w.pg is par:
                for d in w.pre:
                    deps[id(d)] = d
            else:
                cur = list(w.lw) + list(w.rd_eng.values()) + list(w.rd_dma)
                for d in cur:
                    deps[id(d)] = d
                if par is not None:
                    w.pre = cur
        keep = []
        for d in deps.values():
            if d.dma:
                keep.append(d)
            elif d.eng != eng:
                d.signal = True
                keep.append(d)
            elif dma or (SAME_ENG_SYNC and eng != "pe"):
                d.signal = True
                keep.append(d)
        o.deps = keep
        for r in reads:
            if dma:
                r.rd_dma.append(o)
            else:
                r.rd_eng[eng] = o
        for w in writes:
            if par is not None and w.pg is par:
                w.lw.append(o)
            else:
                w.lw = [o]
                w.rd_eng = {}
                w.rd_dma = []
                w.pg = par
        self.q[eng].append(o)
        return o

    def dma(self, eng, out, in_, reads=(), writes=(), par=None):
        return self.op(eng, lambda e: e.dma_start(out=out, in_=in_), reads, writes, dma=True, par=par)

    def mm(self, out, lhsT, rhs, start, stop, reads, writes, skip=False):
        if skip:
            return self.op("pe", lambda e: e.matmul(out, lhsT=lhsT, rhs=rhs, start=start, stop=stop,
                                                    skip_group_check=True), reads, writes)
        return self.op("pe", lambda e: e.matmul(out, lhsT=lhsT, rhs=rhs, start=start, stop=stop), reads, writes)

    def tr(self, out, in_, ident, reads, writes):
        return self.op("pe", lambda e: e.transpose(out=out, in_=in_, identity=ident), reads, writes)

    def act(self, out, in_, func, reads, writes, eng="act", **kw):
        return self.op("act", lambda e: e.activation(out=out, in_=in_, func=func, **kw), reads, writes)

    def cp(self, eng, out, in_, reads, writes):
        if eng == "act":
            return self.op("act", lambda e: e.copy(out=out, in_=in_), reads, writes)
        return self.op(eng, lambda e: e.tensor_copy(out=out, in_=in_), reads, writes)

    def ts(self, eng, out, in0, s1, s2, op0, op1, reads, writes):
        if op1 is None:
            return self.op(eng, lambda e: e.tensor_scalar(out=out, in0=in0, scalar1=s1, scalar2=None, op0=op0),
                           reads, writes)
        return self.op(eng, lambda e: e.tensor_scalar(out=out, in0=in0, scalar1=s1, scalar2=s2, op0=op0, op1=op1),
                       reads, writes)

    def tt(self, eng, out, in0, in1, op, reads, writes):
        return self.op(eng, lambda e: e.tensor_tensor(out=out, in0=in0, in1=in1, op=op), reads, writes)

    def stt(self, out, in0, scalar, in1, op0, op1, reads, writes):
        return self.op("dve", lambda e: e.scalar_tensor_tensor(out=out, in0=in0, scalar=scalar, in1=in1,
                                                               op0=op0, op1=op1), reads, writes)

    def memset(self, eng, out, val, writes):
        return self.op(eng, lambda e: e.memset(out, val), (), writes)

    def flush(self):
        nc = self.nc
        for e in self.ENGS:
            c = self.ecount[e]
            last = None
            for o in self.q[e]:
                if not o.dma:
                    last = o
            if last is not None:
                last.signal = True
            for o in self.q[e]:
                if o.dma:
                    k = self.dk[e]
                    o.sem = self.dsem[e][k % N_DMA_SEMS]
                    o.val = 16 * (k // N_DMA_SEMS + 1)
                    self.dk[e] = k + 1
                elif o.signal:
                    c += 1
                    o.count = c
            self.ecount[e] = c
        fin = []
        for e in self.ENGS:
            if e != "sp":
                fin.append((self.esem[e], self.ecount[e]))
        for e in ("sp", "pool"):
            k = self.dk[e]
            for i in range(N_DMA_SEMS):
                n = (k - i + N_DMA_SEMS - 1) // N_DMA_SEMS if k > i else 0
                fin.append((self.dsem[e][i], 16 * n))
        qs = self.q
        self.q = {e: [] for e in self.ENGS}
        esem = self.esem

        def run(e):
            ops = qs[e]

            def body(eng):
                waited = {}
                for o in ops:
                    ws = []
                    for d in o.deps:
                        if d.dma:
                            ws.append((d.sem, d.val))
                        else:
                            ws.append((esem[d.eng], d.count))
                    if o.dma and o.val > 16:
                        ws.append((o.sem, o.val - 16))
                    for s, v in ws:
                        if waited.get(id(s), 0) < v:
                            eng.wait_ge(s, v)
                            waited[id(s)] = v
                    inst = o.fn(eng)
                    if o.dma:
                        inst.then_inc(o.sem, 16)
                    elif o.signal:
                        inst.then_inc(esem[e], 1)
                for s, v in fin:
                    if v > 0 and waited.get(id(s), 0) < v:
                        eng.wait_ge(s, v)
            return body

        import os as _os
        if _os.environ.get("DUMP"):
            names = {id(v): "c_" + k for k, v in esem.items()}
            for q_, lst in self.dsem.items():
                for i, v in enumerate(lst):
                    names[id(v)] = "d_%s%d" % (q_, i)
            for e in self.ENGS:
                waited = {}
                out = []
                for o in qs[e]:
                    ws = []
                    for d in o.deps:
                        ws.append((d.sem, d.val) if d.dma else (esem[d.eng], d.count))
                    if o.dma and o.val > 16:
                        ws.append((o.sem, o.val - 16))
                    for s_, v in ws:
                        if waited.get(id(s_), 0) < v:
                            out.append("W %s>=%d" % (names[id(s_)], v))
                            waited[id(s_)] = v
                    if o.dma:
                        out.append("DMA +%s=%d" % (names[id(o.sem)], o.val))
                    elif o.signal:
                        out.append("OP +c_%s=%d" % (e, o.count))
                    else:
                        out.append("OP")
                for s_, v in fin:
                    if v > 0 and waited.get(id(s_), 0) < v:
                        out.append("FW %s>=%d" % (names[id(s_)], v))
                print("ENG", e, len(qs[e]), " ".join(out[:400]))
        with nc.Block() as block:
            for e in self.ENGS:
                getattr(block, self.OBJ[e])(run(e))


def _bucket(d):
    d = np.asarray(d)
    n = np.maximum(d, 0)
    large = 16 + (np.log(np.maximum(n, 1).astype(np.float32) / 16) / math.log(128 / 16) * 16).astype(np.int32)
    large = np.minimum(large, 31)
    return np.where(n < 16, n, large)


LV = 416
VOFF = 160


def make_consts(S):
    NT = S // 128
    c = {}
    c["identf"] = np.eye(128, dtype=np.float32)
    k = np.arange(128)[:, None]
    q = np.arange(128)[None, :]
    c["tri"] = (q >= k).astype(np.float32)
    c["wmask"] = (k > q).astype(np.float32)
    d = np.arange(LV) - VOFF
    oh = np.zeros((33, LV), np.float32)
    b = _bucket(d)
    for i in range(LV):
        if d[i] >= 0:
            oh[b[i], i] = 1.0
        else:
            oh[32, i] = 1.0
    c["oh"] = oh
    zz = np.zeros((32, 640), np.float32)
    for j in range(16):
        zz[j, 256 + j] = 1.0
        zz[16 + j, 256 + j] = 1.0
    c["zz"] = zz
    t = np.arange(S)
    c["eblk"] = (t[None, :] // 64 == np.arange(64)[:, None]).astype(np.float32)
    ncmp = (S - 32) // 16 + 1
    cs = np.arange(ncmp) * 16
    ss = np.arange(S // 64) * 64
    ov = np.maximum(np.minimum(cs[:, None] + 32, ss[None, :] + 64) - np.maximum(cs[:, None], ss[None, :]), 0) / 32.0
    ovp = np.zeros((((ncmp + 127) // 128) * 128, 64), np.float32)
    ovp[:ncmp, :S // 64] = ov
    c["ov"] = ovp
    M = np.zeros((NT, 128, 64), np.float32)
    A = np.zeros((NT, 128, 64), np.float32)
    for qt in range(NT):
        qpos = qt * 128 + np.arange(128)
        qb = qpos // 64
        n = np.arange(64)[None, :]
        forced = (n == 0) | (n == qb[:, None]) | (n == qb[:, None] - 1)
        future = n > qb[:, None]
        M[qt] = (~forced & ~future).astype(np.float32)
        A[qt] = np.where(forced, 1e30, np.where(future, -1e30, 0.0))
    c["mtk"] = M
    c["atk"] = A
    p = np.arange(128)
    inv = (10000.0 ** (-(np.arange(16, dtype=np.float32)) / 16)).astype(np.float32)
    c["invf"] = inv[p % 16][:, None].astype(np.float32)
    c["sgn"] = np.where(p % 32 < 16, -1.0, 1.0).astype(np.float32)[:, None]
    xd = np.zeros((128, 4, 8), np.float32)
    xd[:, 0, :] = 384.0
    for qt in range(1, 4):
        xd[:, qt, :] = (127 - np.arange(128) + (3 - qt) * 128)[:, None]
    c["xdst"] = xd.reshape(128, 32)
    return c


CONST_SHAPES = None


def build(S=4096, L=2, dbg=(), stop_after=None, NBC=1):
    NT = S // 128
    NB = S // 512
    NCMP = (S - 32) // 16 + 1
    NCT = (NCMP + 127) // 128
    NBLK = S // 64
    nc = bass.Bass("TRN2", target_bir_lowering=False)
    consts = make_consts(S)

    def din(name, shape, dt=F32):
        return nc.dram_tensor(name, list(shape), dt, kind="ExternalInput")

    def dscr(name, shape, dt=F32):
        kind = "ExternalOutput" if name in dbg else "Internal"
        return nc.dram_tensor(name, list(shape), dt, kind=kind)

    x_in = din("x", [NBC, S, 1024])
    p_in = din("p", [L, NBC, S, 256])
    pos_in = din("pos", [NBC, S], I32)
    relb = din("relb", [32, 8])
    class Lazy(dict):
        def __init__(self, fn):
            super().__init__()
            self.fn = fn

        def __missing__(self, k):
            self[k] = self.fn(k)
            return self[k]

    cd = Lazy(lambda k: din("c_" + k, consts[k].shape))
    WSH = dict(g_pre=[1, 1024], g_post=[1, 1024], g_fpre=[1, 1024], g_fpost=[1, 1024],
               w_fm=[1024, 1024], w_kr=[1024, 64], w_tm=[1024, 664], qn_g=[1, 256], kvn_g=[1, 128],
               wuq_n=[256, 512], wuq_r=[256, 512], wukv_k=[128, 512], wukv_v=[128, 512],
               cmp_pos=[2, 64, 32], cmp_w1=[2, 64, 32 * 128], cmp_w2=[2, 128, 64],
               w_o=[1024, 1024], w_gate=[1024, 2816], w_up=[1024, 2816], conv_w=[128, 22 * 3],
               conv_b=[128, 22], w_down=[2816, 1024], ple_proj=[256, 1024], ple_gate=[1024, 1024])
    W = [Lazy(lambda k, l=l: din("l%d_%s" % (l, k), WSH[k])) for l in range(L)]
    out_d = nc.dram_tensor("out", [NBC, S, 1024], F32, kind="ExternalOutput")

    cs_d = dscr("cs", [2, 128, S])
    rep_d = dscr("rep", [8, 128, LV])
    eb_d = dscr("eb", [2, 2, 128, 512], BF16)
    bc_d = dscr("bc", [2, 32, 512], BF16)
    xden_d = dscr("xden", [128, 32])
    fmT = dscr("fmT", [1024, S], BF16)
    krT = dscr("krT", [32, S], BF16)
    vnsa = dscr("vnsa", [S, 256], BF16)
    gates_d = dscr("gates", [S, 24])
    qnT = dscr("qnT", [512, S], BF16)
    qrT = dscr("qrT", [256, S], BF16)
    knT = dscr("knT", [512, S], BF16)
    vmla = dscr("vmla", [S, 512], BF16)
    kcT = dscr("kcT", [2, 64, NCT * 128], BF16)
    vc_d = dscr("vc", [2, NCT * 128, 64], BF16)
    o_all = dscr("o_all", [S, 1024], BF16)
    xs1 = dscr("xs1", [S, 1024])
    xs2 = dscr("xs2", [S, 1024])
    xs3 = dscr("xs3", [S, 1024])
    aT_d = dscr("aT", [2816, S], BF16)

    with contextlib.ExitStack() as top:
        Sc = Sched(nc, top)
        rr = [0]

        def ev_eng():
            rr[0] += 1
            return "act" if rr[0] % 2 else "dve"

        def dq():
            rr[0] += 1
            return "sp" if (rr[0] % 2 or not USE_POOL_DMA) else "pool"

        def dma_split(eng, out_ap, in_ap, axis, step, reads=(), writes=()):
            n = out_ap.shape[axis]
            tok = object()
            for a in range(0, n, step):
                b = min(n, a + step)
                idx = [slice(None)] * len(out_ap.shape)
                idx[axis] = slice(a, b)
                Sc.dma(eng, out_ap[tuple(idx)], in_ap[tuple(idx)], reads=reads, writes=writes, par=tok)

        class Scope:
            def __init__(self):
                self.st = contextlib.ExitStack()
                self.n = 0

            def __enter__(self):
                self.st.__enter__()
                return self

            def __exit__(self, *a):
                Sc.flush()
                return self.st.__exit__(*a)

            def sb(self, shape, dt=F32):
                self.n += 1
                t = self.st.enter_context(nc.sbuf_tensor("t%d_%d" % (Sc.nops, self.n), list(shape), dt))
                return t, Res()

            def ps(self, shape=(128, 512), dt=F32):
                self.n += 1
                t = self.st.enter_context(nc.psum_tensor("p%d_%d" % (Sc.nops, self.n), list(shape), dt))
                return t, Res()

        def bcast_rows(dram, row0, nrows_p, ncols):
            return bass.AP(dram, row0 * ncols, [[0, nrows_p], [1, ncols]])

        def load_cast(sc, dst, rdst, src_ap, shape, eng=None, stage=None):
            if stage is None:
                stage = sc.sb(shape, F32)
            stg, rstg = stage
            sv = stg[:]
            if len(src_ap.shape) == 3:
                if tuple(sv.shape) != tuple(src_ap.shape):
                    sv = stg[:, 0:src_ap.shape[1], 0:src_ap.shape[2]]
                dma_split(dq(), sv, src_ap, 1, 1, writes=[rstg])
            else:
                Sc.dma(dq(), sv, src_ap, writes=[rstg])
            Sc.cp(eng or ev_eng(), dst, sv, [rstg], [rdst])

        def rms_rstd(sc, src_ap, n, junk, rjunk, reads):
            ss, rss = sc.sb([128, 1])
            Sc.op("dve", lambda e: e.scalar_tensor_tensor(out=junk, in0=src_ap, scalar=1.0, in1=src_ap,
                                                           op0=ALU.mult, op1=ALU.mult, accum_out=ss[:]),
                  reads, [rjunk, rss])
            Sc.ts("dve", ss[:], ss[:], 1.0 / n, EPS, ALU.mult, ALU.add, [rss], [rss])
            Sc.act(ss[:], ss[:], AF.Sqrt, [rss], [rss])
            Sc.op("dve", lambda e: e.reciprocal(out=ss[:], in_=ss[:]), [rss], [rss])
            return ss, rss

        for bi_ in range(NBC):
            import os as _os0
            with (Scope() if not _os0.environ.get("SKIP_P0") else contextlib.nullcontext()) as sc:
              if sc is not None:
                  posi, r_posi = sc.sb([128, S], I32)
                  ang, r_ang = sc.sb([128, S])
                  u, r_u = sc.sb([128, S])
                  ki, r_ki = sc.sb([128, S], I32)
                  m, r_m = sc.sb([128, S])
                  invf, r_invf = sc.sb([128, 1])
                  sgn, r_sgn = sc.sb([128, 1])
                  Sc.dma("sp", posi[:], bcast_rows(pos_in, bi_, 128, S), writes=[r_posi])
                  Sc.dma("sp", invf[:], cd["invf"].ap(), writes=[r_invf])
                  Sc.dma("sp", sgn[:], cd["sgn"].ap(), writes=[r_sgn])
                  Sc.cp("dve", ang[:], posi[:], [r_posi], [r_ang])
                  Sc.ts("dve", ang[:], ang[:], invf[:, 0:1], None, ALU.mult, None, [r_ang, r_invf], [r_ang])
                  TWO_PI = 2.0 * math.pi
                  for which in range(2):
                      shift = math.pi / 2 if which == 0 else 0.0
                      Sc.ts("dve", u[:], ang[:], shift, 1.0 / TWO_PI, ALU.add, ALU.mult, [r_ang], [r_u])
                      Sc.cp("dve", ki[:], u[:], [r_u], [r_ki])
                      Sc.cp("dve", m[:], ki[:], [r_ki], [r_m])
                      Sc.stt(u[:], m[:], -TWO_PI, ang[:], ALU.mult, ALU.add, [r_m, r_ang], [r_u])
                      if shift:
                          Sc.ts("dve", u[:], u[:], shift, None, ALU.add, None, [r_u], [r_u])
                      Sc.ts("dve", m[:], u[:], math.pi, None, ALU.is_gt, None, [r_u], [r_m])
                      Sc.stt(u[:], m[:], -TWO_PI, u[:], ALU.mult, ALU.add, [r_m, r_u], [r_u])
                      Sc.ts("dve", m[:], u[:], -math.pi, None, ALU.is_lt, None, [r_u], [r_m])
                      Sc.stt(u[:], m[:], TWO_PI, u[:], ALU.mult, ALU.add, [r_m, r_u], [r_u])
                      Sc.ts("dve", u[:], u[:], -3.1415925, 3.1415925, ALU.max, ALU.min, [r_u], [r_u])
                      Sc.act(m[:], u[:], AF.Sin, [r_u], [r_m])
                      if which == 1:
                          Sc.ts("dve", m[:], m[:], sgn[:, 0:1], None, ALU.mult, None, [r_m, r_sgn], [r_m])
                      Sc.dma("sp", cs_d.ap()[which], m[:], reads=[r_m])

                  P0S = _os0.environ.get('P0_STOP', '')
                  Sc.mute = (P0S == 'a')
                  tb, r_tb = sc.sb([33, 8])
                  t31, r_t31 = sc.sb([32, 8])
                  Sc.memset("dve", tb[:], -4000.0, [r_tb])
                  oh, r_oh = sc.sb([33, LV])
                  pv, r_pv = sc.ps([128, 512])
                  sv, r_sv = sc.sb([128, LV])
                  r_rep = Res()
                  Sc.dma("sp", tb[0:32, :], relb.ap(), writes=[r_tb])
                  Sc.dma("sp", t31[:], bcast_rows(relb, 31, 32, 8), writes=[r_t31])
                  Sc.dma("sp", oh[:], cd["oh"].ap(), writes=[r_oh])
                  Sc.tt("dve", tb[0:32, :], tb[0:32, :], t31[:], ALU.subtract, [r_tb, r_t31], [r_tb])
                  ohb, r_ohb = sc.sb([33, LV], BF16)
                  Sc.cp("dve", ohb[:], oh[:], [r_oh], [r_ohb])
                  tbh, r_tbh = sc.sb([33, 8], BF16)
                  tbf, r_tbf = sc.sb([33, 8])
                  tbl_, r_tbl = sc.sb([33, 8], BF16)
                  Sc.cp("dve", tbh[:], tb[:], [r_tb], [r_tbh])
                  Sc.cp("dve", tbf[:], tbh[:], [r_tbh], [r_tbf])
                  Sc.tt("dve", tbf[:], tb[:], tbf[:], ALU.subtract, [r_tb, r_tbf], [r_tbf])
                  Sc.cp("dve", tbl_[:], tbf[:], [r_tbf], [r_tbl])
                  lwh, r_lwh = sc.sb([33, 128], BF16)
                  lwl, r_lwl = sc.sb([33, 128], BF16)
                  for h in range(8):
                      Sc.cp("dve", lwh[:], tbh[:, h:h + 1].to_broadcast([33, 128]), [r_tbh], [r_lwh])
                      Sc.cp("dve", lwl[:], tbl_[:, h:h + 1].to_broadcast([33, 128]), [r_tbl], [r_lwl])
                      Sc.mm(pv[:, 0:LV], lwh[:], ohb[:], True, False, [r_lwh, r_ohb], [r_pv])
                      Sc.mm(pv[:, 0:LV], lwl[:], ohb[:], False, True, [r_lwl, r_ohb], [r_pv])
                      Sc.cp("act", sv[:], pv[:, 0:LV], [r_pv], [r_sv])
                      Sc.dma("sp", rep_d.ap()[h], sv[:], reads=[r_sv], writes=[r_rep])
                  Sc.mute = Sc.mute or (P0S == 'b')
                  wmf, r_wmf = sc.sb([128, 128])
                  wmb, r_wmb = sc.sb([128, 128], BF16)
                  xd, r_xd = sc.sb([128, 32])
                  e0, r_e0 = sc.sb([128, 8])
                  e0h, r_e0h = sc.sb([128, 8], BF16)
                  e0f, r_e0f = sc.sb([128, 8])
                  e0l, r_e0l = sc.sb([128, 8], BF16)
                  Sc.dma("sp", wmf[:], cd["wmask"].ap(), writes=[r_wmf])
                  Sc.cp("dve", wmb[:], wmf[:], [r_wmf], [r_wmb])
                  Sc.dma("sp", xd[:], cd["xdst"].ap(), writes=[r_xd])
                  Sc.mm(pv[:, 0:8], ohb[:, VOFF:VOFF + 128], tbh[:], True, False, [r_ohb, r_tbh], [r_pv])
                  Sc.mm(pv[:, 0:8], ohb[:, VOFF:VOFF + 128], tbl_[:], False, True, [r_ohb, r_tbl], [r_pv])
                  Sc.act(e0[:], pv[:, 0:8], AF.Exp, [r_pv], [r_e0])
                  Sc.cp("dve", e0h[:], e0[:], [r_e0], [r_e0h])
                  Sc.cp("dve", e0f[:], e0h[:], [r_e0h], [r_e0f])
                  Sc.tt("dve", e0f[:], e0[:], e0f[:], ALU.subtract, [r_e0, r_e0f], [r_e0f])
                  Sc.cp("dve", e0l[:], e0f[:], [r_e0f], [r_e0l])
                  Sc.mm(pv[:, 0:8], wmb[:], e0h[:], True, False, [r_wmb, r_e0h], [r_pv])
                  Sc.mm(pv[:, 0:8], wmb[:], e0l[:], False, True, [r_wmb, r_e0l], [r_pv])
                  Sc.tt("dve", xd[:, 0:8], xd[:, 0:8], pv[:, 0:8], ALU.add, [r_xd, r_pv], [r_xd])
                  Sc.dma("sp", xden_d.ap(), xd[:], reads=[r_xd])
                  Sc.mute = Sc.mute or (P0S == 'c')
                  tri, r_tri = sc.sb([128, 128])
                  Sc.dma("sp", tri[:], cd["tri"].ap(), writes=[r_tri])
                  for dl in range(2):
                      for hk in range(2):
                          tl, r_tl = sc.sb([128, 512])
                          eb, r_eb = sc.sb([128, 512], BF16)
                          for g in range(4):
                              h = hk * 4 + g
                              src = bass.AP(rep_d, h * 128 * LV + 128 * dl + VOFF, [[LV - 1, 128], [1, 128]])
                              Sc.dma("sp", tl[:, g * 128:(g + 1) * 128], src, reads=[r_rep], writes=[r_tl])
                          Sc.act(tl[:], tl[:], AF.Exp, [r_tl], [r_tl])
                          if dl == 0:
                              for g in range(4):
                                  Sc.tt("dve", tl[:, g * 128:(g + 1) * 128], tl[:, g * 128:(g + 1) * 128], tri[:],
                                        ALU.mult, [r_tl, r_tri], [r_tl])
                          Sc.cp("dve", eb[:], tl[:], [r_tl], [r_eb])
                          Sc.dma("sp", eb_d.ap()[dl, hk], eb[:], reads=[r_eb])
                  for hk in range(2):
                      bt, r_bt = sc.sb([16, 512])
                      bhi, r_bhi = sc.sb([16, 512], BF16)
                      bhf, r_bhf = sc.sb([16, 512])
                      blo, r_blo = sc.sb([16, 512], BF16)
                      for g in range(4):
                          h = hk * 4 + g
                          src = bass.AP(rep_d, h * 128 * LV + 97 + VOFF, [[LV - 16, 16], [1, 128]])
                          Sc.dma("sp", bt[:, g * 128:(g + 1) * 128], src, reads=[r_rep], writes=[r_bt])
                      Sc.ts("dve", bt[:], bt[:], 8.0, None, ALU.mult, None, [r_bt], [r_bt])
                      Sc.cp("dve", bhi[:], bt[:], [r_bt], [r_bhi])
                      Sc.cp("dve", bhf[:], bhi[:], [r_bhi], [r_bhf])
                      Sc.tt("dve", bhf[:], bt[:], bhf[:], ALU.subtract, [r_bt, r_bhf], [r_bhf])
                      Sc.cp("dve", blo[:], bhf[:], [r_bhf], [r_blo])
                      Sc.dma("sp", bc_d.ap()[hk, 0:16, :], bhi[:], reads=[r_bhi])
                      Sc.dma("sp", bc_d.ap()[hk, 16:32, :], blo[:], reads=[r_blo])
            Sc.mute = False
            if stop_after == "P0":
                return nc

            x_src = x_in.ap()[bi_]
            for l in range(L):
                Wl = W[l]
                x_dst = out_d.ap()[bi_] if l == L - 1 else xs3.ap()
                with Scope() as sc:
                    identf, r_idf = sc.sb([128, 128])
                    identb, r_idb = sc.sb([128, 128], BF16)
                    Sc.dma("sp", identf[:], cd["identf"].ap(), writes=[r_idf])
                    Sc.cp("dve", identb[:], identf[:], [r_idf], [r_idb])
                    gpre, r_gpre = sc.sb([128, 1024])
                    qng, r_qng = sc.sb([128, 256])
                    kvng, r_kvng = sc.sb([128, 128])
                    Sc.dma("sp", gpre[:], bcast_rows(Wl["g_pre"], 0, 128, 1024), writes=[r_gpre])
                    Sc.dma("sp", qng[:], bcast_rows(Wl["qn_g"], 0, 128, 256), writes=[r_qng])
                    Sc.dma("sp", kvng[:], bcast_rows(Wl["kvn_g"], 0, 128, 128), writes=[r_kvng])
                    stage = sc.sb([128, 8, 1024])
                    wfm, r_wfm = sc.sb([128, 8, 1024], BF16)
                    load_cast(sc, wfm[:], r_wfm, Wl["w_fm"].ap().rearrange("(k p) n -> p k n", p=128), None, stage=stage)
                    wtm, r_wtm = sc.sb([128, 8, 664], BF16)
                    load_cast(sc, wtm[:], r_wtm, Wl["w_tm"].ap().rearrange("(k p) n -> p k n", p=128), [128, 8, 664])
                    wkr, r_wkr = sc.sb([128, 8, 64], BF16)
                    load_cast(sc, wkr[:], r_wkr, Wl["w_kr"].ap().rearrange("(k p) n -> p k n", p=128), [128, 8, 64])
                    wuqn, r_wuqn = sc.sb([128, 2, 512], BF16)
                    load_cast(sc, wuqn[:], r_wuqn, Wl["wuq_n"].ap().rearrange("(k p) n -> p k n", p=128), [128, 2, 512])
                    wuqr, r_wuqr = sc.sb([128, 2, 512], BF16)
                    load_cast(sc, wuqr[:], r_wuqr, Wl["wuq_r"].ap().rearrange("(k p) n -> p k n", p=128), [128, 2, 512])
                    wukk, r_wukk = sc.sb([128, 512], BF16)
                    load_cast(sc, wukk[:], r_wukk, Wl["wukv_k"].ap(), [128, 512])
                    wukv, r_wukv = sc.sb([128, 512], BF16)
                    load_cast(sc, wukv[:], r_wukv, Wl["wukv_v"].ap(), [128, 512])

                    xt = [sc.sb([128, 1024]) for _ in range(2)]
                    junk, r_junk = sc.sb([128, 1024], BF16)
                    hb = [sc.sb([128, 1024], BF16) for _ in range(2)]
                    ptr = [sc.ps([128, 1024], BF16) for _ in range(2)]
                    hT = [sc.sb([128, 8, 512], BF16) for _ in range(2)]
                    pmm = [sc.ps() for _ in range(4)]
                    pq = sc.ps([128, 1024], BF16)
                    c1 = [sc.sb([128, 384]) for _ in range(2)]
                    fo = [sc.sb([128, 512], BF16) for _ in range(3)]
                    cost, r_cost = sc.sb([128, 512])
                    sint, r_sint = sc.sb([128, 512])
                    t1 = [sc.sb([128, 512]) for _ in range(2)]
                    cqn = [sc.sb([128, 384], BF16) for _ in range(2)]
                    cqnT, r_cqnT = sc.sb([128, 2, 512], BF16)
                    ckvnT, r_ckvnT = sc.sb([128, 512], BF16)
                    vo = [sc.sb([128, 256], BF16) for _ in range(2)]
                    go = [sc.sb([128, 24]) for _ in range(2)]
                    vm = [sc.sb([128, 512], BF16) for _ in range(2)]
                    nfo = [0]

                    def evac_store(psum_ap, rps, dst_ap, np_=128):
                        f, rf = fo[nfo[0] % 3]
                        nfo[0] += 1
                        Sc.cp(ev_eng(), f[0:np_, :], psum_ap, [rps], [rf])
                        Sc.dma(dq(), dst_ap, f[0:np_, :], reads=[rf])

                    def rope_combine(pa, rpa, pb, rpb, np_, dst_ap):
                        (a, ra), (b, rb) = t1
                        Sc.tt("dve", a[0:np_, :], pa[0:np_, :], cost[0:np_, :], ALU.mult, [rpa, r_cost], [ra])
                        Sc.tt("dve", b[0:np_, :], pb[0:np_, :], sint[0:np_, :], ALU.mult, [rpb, r_sint], [rb])
                        f, rf = fo[nfo[0] % 3]
                        nfo[0] += 1
                        Sc.tt("pool", f[0:np_, :], a[0:np_, :], b[0:np_, :], ALU.add, [ra, rb], [rf])
                        Sc.dma(dq(), dst_ap, f[0:np_, :], reads=[rf])

                    import os as _os
                    A_STOP = _os.environ.get("A_STOP", "")
                    for tb_ in range(NB if A_STOP != "w" else 0):
                        c0 = tb_ * 512
                        hTt, r_hT = hT[tb_ % 2]
                        Sc.dma("sp", cost[:], cs_d.ap()[0, :, c0:c0 + 512], writes=[r_cost])
                        Sc.dma(dq(), sint[:], cs_d.ap()[1, :, c0:c0 + 512], writes=[r_sint])
                        for i in range(4):
                            ti = tb_ * 4 + i
                            (x_t, r_x) = xt[ti % 2]
                            (h_b, r_hb) = hb[ti % 2]
                            (p_t, r_pt) = ptr[ti % 2]
                            Sc.dma(dq(), x_t[:], x_src[ti * 128:(ti + 1) * 128, :], writes=[r_x])
                            ss, rss = rms_rstd(sc, x_t[:], 1024, junk[:], r_junk, [r_x])
                            Sc.stt(h_b[:], x_t[:], ss[:, 0:1], gpre[:], ALU.mult, ALU.mult, [r_x, rss, r_gpre], [r_hb])
                            for k in range(8):
                                Sc.tr(p_t[:, k * 128:(k + 1) * 128], h_b[:, k * 128:(k + 1) * 128], identb[:],
                                      [r_hb, r_idb], [r_pt])
                            Sc.cp("act", hTt[:, :, i * 128:(i + 1) * 128],
                                  p_t[:].rearrange("p (k t) -> p k t", k=8), [r_pt], [r_hT])
                        if A_STOP == "h":
                            continue
                        for c in range(8):
                            pm, rpm = pmm[c % 2]
                            for k in range(8):
                                Sc.mm(pm[:], wfm[:, k, c * 128:(c + 1) * 128], hTt[:, k, :], k == 0, k == 7,
                                      [r_wfm, r_hT], [rpm])
                            evac_store(pm[:], rpm, fmT.ap()[c * 128:(c + 1) * 128, c0:c0 + 512])
                        if A_STOP == "f1":
                            continue
                        (pa, rpa), (pb, rpb) = pmm[2], pmm[3]
                        for k in range(8):
                            Sc.mm(pa[0:32, :], wkr[:, k, 0:32], hTt[:, k, :], k == 0, k == 7, [r_wkr, r_hT], [rpa])
                        for k in range(8):
                            Sc.mm(pb[0:32, :], wkr[:, k, 32:64], hTt[:, k, :], k == 0, k == 7, [r_wkr, r_hT], [rpb])
                        rope_combine(pa, rpa, pb, rpb, 32, krT.ap()[:, c0:c0 + 512])
                        if A_STOP == "f":
                            continue
                        for i in range(4):
                            ti = tb_ * 4 + i
                            (p1, rp1), (p2, rp2) = pmm[0], pmm[1]
                            T2M = _os.environ.get("T2M", "")
                            for k in range(8 if T2M != "nop1" else 0):
                                Sc.mm(p1[:, 0:384], hTt[:, k, i * 128:(i + 1) * 128], wtm[:, k, 0:384], k == 0, k == 7,
                                      [r_hT, r_wtm], [rp1])
                            for k in range(8 if T2M != "nop2" else 0):
                                Sc.mm(p2[:, 0:280], hTt[:, k, i * 128:(i + 1) * 128], wtm[:, k, 384:664], k == 0, k == 7,
                                      [r_hT, r_wtm], [rp2])
                            if A_STOP == "t2":
                                continue
                            cq, r_cq = cqn[ti % 2]
                            c_1, r_c1 = c1[ti % 2]
                            T1M = int(_os.environ.get("T1M", "9"))
                            Sc.cp("act", c_1[:], p1[:, 0:384], [rp1], [r_c1])
                            Sc.mute = T1M <= 1
                            ss, rss = rms_rstd(sc, c_1[:, 0:256], 256, junk[:, 0:256], r_junk, [r_c1])
                            Sc.stt(cq[:, 0:256], c_1[:, 0:256], ss[:, 0:1], qng[:], ALU.mult, ALU.mult,
                                   [r_c1, rss, r_qng], [r_cq])
                            Sc.mute = T1M <= 2
                            ss2, rss2 = rms_rstd(sc, c_1[:, 256:384], 128, junk[:, 0:128], r_junk, [r_c1])
                            Sc.stt(cq[:, 256:384], c_1[:, 256:384], ss2[:, 0:1], kvng[:], ALU.mult, ALU.mult,
                                   [r_c1, rss2, r_kvng], [r_cq])
                            pqt, rpq = pq
                            Sc.mute = T1M <= 3
                            for k in range(3):
                                Sc.tr(pqt[:, k * 128:(k + 1) * 128], cq[:, k * 128:(k + 1) * 128], identb[:],
                                      [r_cq, r_idb], [rpq])
                            Sc.mute = T1M <= 4
                            Sc.cp("act", cqnT[:, :, i * 128:(i + 1) * 128],
                                  pqt[:, 0:256].rearrange("p (k t) -> p k t", k=2), [rpq], [r_cqnT])
                            Sc.mute = T1M <= 5
                            Sc.cp("act", ckvnT[:, i * 128:(i + 1) * 128], pqt[:, 256:384], [rpq], [r_ckvnT])
                            Sc.mute = False
                            if A_STOP == "t1":
                                continue
                            v_o, r_vo = vo[ti % 2]
                            Sc.cp("dve", v_o[:], p2[:, 0:256], [rp2], [r_vo])
                            Sc.dma(dq(), vnsa.ap()[ti * 128:(ti + 1) * 128, :], v_o[:], reads=[r_vo])
                            g_o, r_go = go[ti % 2]
                            Sc.act(g_o[:], p2[:, 256:280], AF.Sigmoid, [rp2], [r_go])
                            Sc.dma(dq(), gates_d.ap()[ti * 128:(ti + 1) * 128, :], g_o[:], reads=[r_go])
                        if A_STOP in ("t", "t1", "t2"):
                            continue
                        for hp in range(4):
                            pm, rpm = pmm[hp % 2]
                            for k in range(2):
                                Sc.mm(pm[:], wuqn[:, k, hp * 128:(hp + 1) * 128], cqnT[:, k, :], k == 0, k == 1,
                                      [r_wuqn, r_cqnT], [rpm])
                            evac_store(pm[:], rpm, qnT.ap()[hp * 128:(hp + 1) * 128, c0:c0 + 512])
                        for grp in range(2):
                            (pa, rpa), (pb, rpb) = pmm[2], pmm[3]
                            for k in range(2):
                                Sc.mm(pa[:], wuqr[:, k, grp * 128:(grp + 1) * 128], cqnT[:, k, :], k == 0, k == 1,
                                      [r_wuqr, r_cqnT], [rpa])
                            for k in range(2):
                                Sc.mm(pb[:], wuqr[:, k, 256 + grp * 128:256 + (grp + 1) * 128], cqnT[:, k, :], k == 0,
                                      k == 1, [r_wuqr, r_cqnT], [rpb])
                            rope_combine(pa, rpa, pb, rpb, 128, qrT.ap()[grp * 128:(grp + 1) * 128, c0:c0 + 512])
                        for hp in range(4):
                            pm, rpm = pmm[hp % 2]
                            Sc.mm(pm[:], wukk[:, hp * 128:(hp + 1) * 128], ckvnT[:], True, True, [r_wukk, r_ckvnT], [rpm])
                            evac_store(pm[:], rpm, knT.ap()[hp * 128:(hp + 1) * 128, c0:c0 + 512])
                        for i in range(4):
                            ti = tb_ * 4 + i
                            pm, rpm = pmm[i % 2]
                            Sc.mm(pm[:], ckvnT[:, i * 128:(i + 1) * 128], wukv[:], True, True, [r_ckvnT, r_wukv], [rpm])
                            v_m, r_vm = vm[ti % 2]
                            Sc.cp(ev_eng(), v_m[:], pm[:], [rpm], [r_vm])
                            Sc.dma(dq(), vmla.ap()[ti * 128:(ti + 1) * 128, :], v_m[:], reads=[r_vm])
                if stop_after == "A%d" % l:
                    return nc

                with Scope() as sc:
                    w1s, r_w1s = sc.sb([64, 32 * 128])
                    w1b_ = [sc.sb([64, 32, 128], BF16) for _ in range(2)]
                    w2s, r_w2s = sc.sb([128, 64])
                    w2b_ = [sc.sb([128, 64], BF16) for _ in range(2)]
                    pos, r_pos = sc.sb([64, 32])
                    posr_ = [sc.sb([64, 32, NCMP], BF16) for _ in range(2)]
                    xT_ = [sc.sb([64, S], BF16) for _ in range(2)]
                    hid_ = [sc.sb([128, NCT * 128], BF16) for _ in range(2)]
                    ph_ = [sc.ps() for _ in range(2)]
                    po_ = [sc.ps() for _ in range(2)]
                    ko, r_ko = sc.sb([64, NCT * 128], BF16)
                    vo_l = [sc.sb([128, 64], BF16) for _ in range(2)]
                    nb_ = 0
                    for kv in range(2):
                        w1b, r_w1b = w1b_[kv]
                        w2b, r_w2b = w2b_[kv]
                        posr, r_posr = posr_[kv]
                        Sc.dma("sp", w1s[:], Wl["cmp_w1"].ap()[kv], writes=[r_w1s])
                        Sc.cp("dve", w1b[:], w1s[:].rearrange("p (l j) -> p l j", l=32), [r_w1s], [r_w1b])
                        Sc.dma("sp", w2s[:], Wl["cmp_w2"].ap()[kv], writes=[r_w2s])
                        Sc.cp("dve", w2b[:], w2s[:], [r_w2s], [r_w2b])
                        Sc.dma("sp", pos[:], Wl["cmp_pos"].ap()[kv], writes=[r_pos])
                        Sc.cp("pool", posr[:], pos[:].unsqueeze(2).to_broadcast([64, 32, NCMP]), [r_pos], [r_posr])
                        for hk in range(2):
                            xT, r_xT = xT_[hk]
                            hid, r_hid = hid_[hk]
                            ph, rph = ph_[hk]
                            r0 = 512 + kv * 128 + hk * 64
                            Sc.dma("sp", xT[:], fmT.ap()[r0:r0 + 64, :], writes=[r_xT])
                            for li in range(32):
                                rhs = xT[:, li:li + 16 * (NCMP - 1) + 1:16]
                                Sc.mm(ph[:, 0:NCMP], w1b[:, li, :], rhs, li == 0, False, [r_w1b, r_xT], [rph])
                                Sc.mm(ph[:, 0:NCMP], w1b[:, li, :], posr[:, li, :], False, li == 31, [r_w1b, r_posr], [rph])
                            Sc.memset("dve", hid[:], 0.0, [r_hid])
                            Sc.act(hid[:, 0:NCMP], ph[:, 0:NCMP], AF.Gelu_apprx_tanh, [rph], [r_hid])
                            if kv == 0:
                                po, rpo = po_[nb_ % 2]
                                nb_ += 1
                                Sc.mm(po[0:64, 0:NCT * 128], w2b[:], hid[:], True, True, [r_w2b, r_hid], [rpo])
                                Sc.cp("dve", ko[:], po[0:64, 0:NCT * 128], [rpo], [r_ko])
                                Sc.dma("sp", kcT.ap()[hk], ko[:], reads=[r_ko])
                            else:
                                for ct in range(NCT):
                                    po, rpo = po_[nb_ % 2]
                                    vo_, r_vo_ = vo_l[nb_ % 2]
                                    nb_ += 1
                                    Sc.mm(po[:, 0:64], hid[:, ct * 128:(ct + 1) * 128], w2b[:], True, True,
                                          [r_hid, r_w2b], [rpo])
                                    Sc.cp("dve", vo_[:], po[:, 0:64], [rpo], [r_vo_])
                                    Sc.dma("sp", vc_d.ap()[hk, ct * 128:(ct + 1) * 128, :], vo_[:], reads=[r_vo_])
                if stop_after == "B%d" % l:
                    return nc

                for hk in range(2):
                    with Scope() as sc:
                        identf, r_idf = sc.sb([128, 128])
                        Sc.dma("sp", identf[:], cd["identf"].ap(), writes=[r_idf])
                        ksT, r_ksT = sc.sb([128, S], BF16)
                        Sc.dma("sp", ksT[0:64, :], fmT.ap()[768 + hk * 64:768 + hk * 64 + 64, :], writes=[r_ksT])
                        est, r_est = sc.sb([128, S])
                        Sc.dma(dq(), est[64:128, :], cd["eblk"].ap(), writes=[r_est])
                        Sc.cp("dve", ksT[64:128, :], est[64:128, :], [r_est], [r_ksT])
                        kwT, r_kwT = sc.sb([64, S], BF16)
                        Sc.dma("sp", kwT[:], fmT.ap()[896 + hk * 64:896 + hk * 64 + 64, :], writes=[r_kwT])
                        kc, r_kc = sc.sb([64, NCT * 128], BF16)
                        Sc.dma("sp", kc[:], kcT.ap()[hk], writes=[r_kc])
                        vs, r_vs = sc.sb([128, NT, 65], BF16)
                        vw, r_vw = sc.sb([128, NT, 65], BF16)
                        Sc.memset("dve", vs[:], 1.0, [r_vs])
                        Sc.memset("dve", vw[:], 1.0, [r_vw])
                        dma_split("sp", vs[:, :, 0:64],
                                  vnsa.ap()[:, hk * 64:hk * 64 + 64].rearrange("(t p) d -> p t d", p=128), 1, 4,
                                  writes=[r_vs])
                        dma_split("sp", vw[:, :, 0:64],
                                  vnsa.ap()[:, 128 + hk * 64:128 + hk * 64 + 64].rearrange("(t p) d -> p t d", p=128),
                                  1, 4, writes=[r_vw])
                        vce, r_vce = sc.sb([128, NCT, 129], BF16)
                        Sc.memset("dve", vce[:], 1.0, [r_vce])
                        Sc.dma("sp", vce[:, :, 0:64], vc_d.ap()[hk].rearrange("(t p) d -> p t d", p=128), writes=[r_vce])
                        ovs, r_ovs = sc.sb([128, NCT, 64])
                        Sc.dma("sp", ovs[:], cd["ov"].ap().rearrange("(t p) d -> p t d", p=128), writes=[r_ovs])
                        Sc.cp("dve", vce[:, :, 65:129], ovs[:], [r_ovs], [r_vce])
                        eb0, r_eb0 = sc.sb([128, 512], BF16)
                        eb1, r_eb1 = sc.sb([128, 512], BF16)
                        Sc.dma("sp", eb0[:], eb_d.ap()[0, hk], writes=[r_eb0])
                        Sc.dma("sp", eb1[:], eb_d.ap()[1, hk], writes=[r_eb1])
                        wms, r_wms = sc.sb([128, 128])
                        wm, r_wm = sc.sb([128, 512], BF16)
                        Sc.dma("sp", wms[:], cd["wmask"].ap(), writes=[r_wms])
                        for g in range(4):
                            Sc.cp("dve", wm[:, g * 128:(g + 1) * 128], wms[:], [r_wms], [r_wm])
                        zzs, r_zzs = sc.sb([32, 640])
                        zz, r_zz = sc.sb([32, 640], BF16)
                        Sc.dma("sp", zzs[:], cd["zz"].ap(), writes=[r_zzs])
                        Sc.cp("dve", zz[:], zzs[:], [r_zzs], [r_zz])
                        bc, r_bc = sc.sb([32, 512], BF16)
                        Sc.dma("sp", bc[:], bc_d.ap()[hk], writes=[r_bc])
                        xdn, r_xdn = sc.sb([128, 32])
                        Sc.dma("sp", xdn[:], xden_d.ap(), writes=[r_xdn])

                        qa = [sc.sb([128, 512], BF16) for _ in range(2)]
                        gt = [sc.sb([128, 24]) for _ in range(2)]
                        mt = [sc.sb([128, 64]) for _ in range(2)]
                        at = [sc.sb([128, 64]) for _ in range(2)]
                        pS = [sc.ps() for _ in range(3)]
                        pc = [sc.ps() for _ in range(2)]
                        po_s, rpo_s = sc.ps()
                        po_w, rpo_w = sc.ps()
                        ptT, rptT = sc.ps([128, 1024], BF16)
                        pT = [sc.sb([128, 512], BF16) for _ in range(3)]
                        oacc = [sc.sb([128, 4, 64]) for _ in range(2)]
                        obf = [sc.sb([128, 256], BF16) for _ in range(2)]
                        imp, r_imp = sc.sb([128, 64])
                        imod, r_imod = sc.sb([128, 64])
                        w1_, r_w1_ = sc.sb([128, 64])
                        w2_, r_w2_ = sc.sb([128, 64])
                        m8, r_m8 = sc.sb([128, 8])
                        selb, r_selb = sc.sb([128, 128], BF16)
                        Sc.memset("dve", selb[:], 0.0, [r_selb])
                        identb, r_idb = sc.sb([128, 128], BF16)
                        Sc.cp("dve", identb[:], identf[:], [r_idf], [r_idb])
                        rin, r_rin = sc.sb([128, 12])
                        riny, r_riny = sc.sb([128, 4])
                        ows = [sc.sb([128, 260]) for _ in range(2)]
                        nps = [0]

                        def score_exp(lhsT, rl, rhs, rr_, np_, extra=None):
                            ps_, rps = pS[nps[0] % 3]
                            pt_, rpt = pT[nps[0] % 3]
                            if not Sc.mute:
                                nps[0] += 1
                            Sc.mm(ps_[0:np_, :], lhsT, rhs, True, extra is None, rl + rr_, [rps])
                            if extra is not None:
                                l2, r2, rl2 = extra
                                Sc.mm(ps_[0:np_, :], l2, r2, False, True, rl2, [rps])
                            Sc.act(pt_[0:np_, :], ps_[0:np_, :], AF.Exp, [rps], [rpt], scale=0.125)
                            return pt_, rpt

                        def qtile(qt, part):
                            q_a, r_qa = qa[qt % 2]
                            g_t, r_gt = gt[qt % 2]
                            m_t, r_mt = mt[qt % 2]
                            a_t, r_at = at[qt % 2]
                            o_a, r_oa = oacc[qt % 2]
                            q0 = qt * 128
                            ow, r_ow = ows[qt % 2]
                            Sc.mute = part != "X"
                            Sc.dma("sp", q_a[0:64, :].rearrange("p (g t) -> p g t", g=4),
                                   fmT.ap()[hk * 256:(hk + 1) * 256, q0:q0 + 128].rearrange("(g p) t -> p g t", p=64),
                                   writes=[r_qa])
                            Sc.dma(dq(), g_t[:], gates_d.ap()[q0:q0 + 128, :], writes=[r_gt])
                            Sc.dma(dq(), m_t[:], cd["mtk"].ap()[qt], writes=[r_mt])
                            Sc.dma(dq(), a_t[:], cd["atk"].ap()[qt], writes=[r_at])
                            nvalid = min(NCMP, 8 * qt + 8)
                            for pr in range(2):
                                Sc.memset("dve", pc[pr][0][:, 0:258], 0.0, [pc[pr][1]])
                            Sc.memset("dve", po_w[:, 0:260], 0.0, [rpo_w])

                            def st1(item):
                                kind, j = item
                                if kind == "c":
                                    M = min(128, nvalid - j * 128)
                                    zoff = 256 + 128 * j - 8 * qt + 8
                                    return score_exp(kc[:, j * 128:j * 128 + M], [r_kc], q_a[0:64, :], [r_qa], M,
                                                     extra=(zz[:, zoff:zoff + M], bc[:], [r_zz, r_bc]))
                                if kind == "w":
                                    kt = qt - j
                                    ptw, rptw = score_exp(kwT[:, kt * 128:(kt + 1) * 128], [r_kwT], q_a[0:64, :], [r_qa], 128)
                                    if j == 0:
                                        Sc.tt("pool", ptw[:], ptw[:], eb0[:], ALU.mult, [rptw, r_eb0], [rptw])
                                    elif j == 1:
                                        Sc.tt("pool", ptw[:], ptw[:], eb1[:], ALU.mult, [rptw, r_eb1], [rptw])
                                    elif j == 4:
                                        Sc.tt("pool", ptw[:], ptw[:], wm[:], ALU.mult, [rptw, r_wm], [rptw])
                                    return ptw, rptw
                                kt = j
                                pts, rpts = score_exp(ksT[:, kt * 128:(kt + 1) * 128], [r_ksT], q_a[:], [r_qa], 128)
                                if qt - kt == 0:
                                    Sc.tt("pool", pts[:], pts[:], eb0[:], ALU.mult, [rpts, r_eb0], [rpts])
                                elif qt - kt == 1:
                                    Sc.tt("pool", pts[:], pts[:], eb1[:], ALU.mult, [rpts, r_eb1], [rpts])
                                return pts, rpts

                            def st2(item, res_):
                                kind, j = item
                                pt_, rpt_ = res_
                                if kind == "c":
                                    M = min(128, nvalid - j * 128)
                                    for g in range(4):
                                        pcc, rpcc = pc[g // 2]
                                        Sc.mm(pcc[:, (g % 2) * 129:(g % 2) * 129 + 129], pt_[0:M, g * 128:(g + 1) * 128],
                                              vce[0:M, j, :], False, False, [rpt_, r_vce], [rpcc], skip=True)
                                elif kind == "w":
                                    kt = qt - j
                                    for g in range(4):
                                        Sc.mm(po_w[:, g * 65:g * 65 + 65], pt_[:, g * 128:(g + 1) * 128], vw[:, kt, :],
                                              False, False, [rpt_, r_vw], [rpo_w], skip=True)
                                else:
                                    for g in range(4):
                                        Sc.mm(po_s[:, g * 65:g * 65 + 65], pt_[:, g * 128:(g + 1) * 128], vs[:, j, :],
                                              False, False, [rpt_, r_vs], [rpo_s], skip=True)

                            def run_pipe(items, depth=2):
                                pend = []
                                for it in items:
                                    pend.append((it, st1(it)))
                                    if len(pend) > depth:
                                        st2(*pend.pop(0))
                                while pend:
                                    st2(*pend.pop(0))

                            c_items = [("c", ct) for ct in range(NCT) if min(128, nvalid - ct * 128) > 0]
                            w_items = [("w", dl) for dl in range(5) if qt - dl >= 0]
                            s_items = [("s", kt) for kt in range(qt + 1)]
                            run_pipe(c_items + w_items)
                            for g in range(4):
                                pcc, rpcc = pc[g // 2]
                                b0 = (g % 2) * 129
                                Sc.ts("dve", rin[:, g:g + 1], pcc[:, b0 + 64:b0 + 65], 1e-30, None, ALU.max, None,
                                      [rpcc], [r_rin])
                            Sc.op("dve", lambda e: e.reciprocal(out=rin[:, 0:4], in_=rin[:, 0:4]), [r_rin], [r_rin])
                            for g in range(4):
                                pcc, rpcc = pc[g // 2]
                                b0 = (g % 2) * 129
                                if g == 0:
                                    Sc.ts("dve", imp[:], pcc[:, b0 + 65:b0 + 129], rin[:, 0:1], None, ALU.mult, None,
                                          [rpcc, r_rin], [r_imp])
                                else:
                                    Sc.stt(imp[:], pcc[:, b0 + 65:b0 + 129], rin[:, g:g + 1], imp[:], ALU.mult, ALU.add,
                                           [rpcc, r_rin, r_imp], [r_imp])
                            Sc.tt("dve", rin[:, 4:8], rin[:, 0:4], g_t[:, hk * 12:hk * 12 + 12:3], ALU.mult, [r_rin, r_gt], [r_rin])
                            for g in range(4):
                                pcc, rpcc = pc[g // 2]
                                b0 = (g % 2) * 129
                                Sc.ts("dve", o_a[:, g, :], pcc[:, b0:b0 + 64], rin[:, 4 + g:5 + g], None, ALU.mult, None,
                                      [rpcc, r_rin], [r_oa])
                            Sc.tt("dve", imod[:], imp[:], m_t[:], ALU.mult, [r_imp, r_mt], [r_imod])
                            Sc.tt("dve", imod[:], imod[:], a_t[:], ALU.add, [r_imod, r_at], [r_imod])
                            Sc.op("dve", lambda e: e.max(out=m8[:], in_=imod[:]), [r_imod], [r_m8])
                            Sc.op("dve", lambda e: e.match_replace(out=w1_[:], in_to_replace=m8[:], in_values=imod[:],
                                                                   imm_value=-3e38), [r_m8, r_imod], [r_w1_])
                            Sc.op("dve", lambda e: e.max(out=m8[:], in_=w1_[:]), [r_w1_], [r_m8])
                            Sc.op("dve", lambda e: e.match_replace(out=w2_[:], in_to_replace=m8[:], in_values=w1_[:],
                                                                   imm_value=-3e38), [r_m8, r_w1_], [r_w2_])
                            Sc.tt("dve", w1_[:], w2_[:], imod[:], ALU.not_equal, [r_w2_, r_imod], [r_w1_])
                            Sc.ts("dve", selb[:, 64:128], w1_[:], -1.0, -NEGB, ALU.add, ALU.mult, [r_w1_], [r_selb])
                            Sc.tr(ptT[:, 0:128], selb[:], identb[:], [r_selb, r_idb], [rptT])
                            for g in range(4):
                                Sc.cp("act", q_a[64:128, g * 128:(g + 1) * 128], ptT[64:128, 0:128],
                                      [rptT], [r_qa])
                            Sc.cp("dve", ow[:], po_w[:, 0:260], [rpo_w], [r_ow])
                            Sc.mute = part != "Y"
                            Sc.memset("dve", po_s[:, 0:260], 0.0, [rpo_s])
                            run_pipe(s_items)

                            for bi, (pp, rpp) in enumerate(((po_s, rpo_s), (ow, r_ow))):
                                if bi == 1 and qt < 4:
                                    c8 = qt * 8 + hk * 4
                                    Sc.tt("dve", riny[:, 0:4], pp[:, 64:260:65], xdn[:, c8:c8 + 4], ALU.add,
                                          [rpp, r_xdn], [r_riny])
                                    Sc.op("dve", lambda e: e.reciprocal(out=riny[:, 0:4], in_=riny[:, 0:4]),
                                          [r_riny], [r_riny])
                                else:
                                    Sc.op("dve", lambda e, pp=pp: e.reciprocal(out=riny[:, 0:4], in_=pp[:, 64:260:65]),
                                          [rpp], [r_riny])
                                Sc.tt("dve", riny[:, 0:4], riny[:, 0:4], g_t[:, hk * 12 + 1 + bi:hk * 12 + 12:3], ALU.mult, [r_riny, r_gt],
                                      [r_riny])
                                for g in range(4):
                                    Sc.stt(o_a[:, g, :], pp[:, g * 65:g * 65 + 64], riny[:, g:g + 1], o_a[:, g, :],
                                           ALU.mult, ALU.add, [rpp, r_riny, r_oa], [r_oa])
                            o_b, r_ob = obf[qt % 2]
                            Sc.cp("act", o_b[:], o_a[:].rearrange("p g d -> p (g d)"), [r_oa], [r_ob])
                            Sc.dma("sp", o_all.ap()[q0:q0 + 128, 512 + hk * 256:512 + (hk + 1) * 256], o_b[:], reads=[r_ob])
                            Sc.mute = False

                        qtile(0, "X")
                        for qt in range(NT):
                            if qt + 1 < NT:
                                qtile(qt + 1, "X")
                            qtile(qt, "Y")
                if stop_after == "C%d" % l:
                    return nc

                with Scope() as sc:
                    tris, r_tris = sc.sb([128, 128])
                    trib, r_trib = sc.sb([128, 128], BF16)
                    Sc.dma("sp", tris[:], cd["tri"].ap(), writes=[r_tris])
                    Sc.cp("dve", trib[:], tris[:], [r_tris], [r_trib])
                    kT = [sc.sb([96, S], BF16) for _ in range(2)]
                    va = [sc.sb([128, NT, 65], BF16) for _ in range(2)]
                    for i in range(2):
                        Sc.memset("dve", va[i][0][:], 1.0, [va[i][1]])
                    qT = [sc.sb([96, 512], BF16) for _ in range(2)]
                    NBUF_D = 5
                    pS = [sc.ps() for _ in range(NBUF_D)]
                    pT = [sc.sb([128, 512], BF16) for _ in range(NBUF_D)]
                    pO = [sc.ps() for _ in range(2)]
                    rin = [sc.sb([128, 4]) for _ in range(2)]
                    ob = [sc.sb([128, 4, 64], BF16) for _ in range(2)]
                    nps_box = [0]
                    nq = 0
                    for h in range(8):
                        k_T, r_kT = kT[h % 2]
                        v_a, r_va = va[h % 2]
                        Sc.dma("sp", k_T[0:64, :], knT.ap()[h * 64:(h + 1) * 64, :], writes=[r_kT])
                        Sc.dma(dq(), k_T[64:96, :], krT.ap(), writes=[r_kT])
                        dma_split("sp", v_a[:, :, 0:64],
                                  vmla.ap()[:, h * 64:(h + 1) * 64].rearrange("(t p) d -> p t d", p=128), 1, 4,
                                  writes=[r_va])
                        for qb in range(NB):
                            q_T, r_qT = qT[nq % 2]
                            p_O, rpO = pO[nq % 2]
                            r_i, r_ri = rin[nq % 2]
                            o_b, r_ob = ob[nq % 2]
                            nq += 1
                            c0 = qb * 512
                            Sc.dma("sp", q_T[0:64, :], qnT.ap()[h * 64:(h + 1) * 64, c0:c0 + 512], writes=[r_qT])
                            Sc.dma(dq(), q_T[64:96, :], qrT.ap()[h * 32:(h + 1) * 32, c0:c0 + 512], writes=[r_qT])
                            Sc.memset("dve", p_O[:, 0:260], 0.0, [rpO])
                            def d1(kt):
                                nonlocal_nps = nps_box[0]
                                nps_box[0] += 1
                                j = kt - 4 * qb
                                qs = max(0, j) * 128
                                ps_, rps = pS[nonlocal_nps % NBUF_D]
                                pt_, rpt = pT[nonlocal_nps % NBUF_D]
                                Sc.mm(ps_[:, qs:512], k_T[:, kt * 128:(kt + 1) * 128], q_T[:, qs:512], True, True,
                                      [r_kT, r_qT], [rps])
                                Sc.act(pt_[:, qs:512], ps_[:, qs:512], AF.Exp, [rps], [rpt], scale=96 ** -0.5)
                                if j >= 0:
                                    Sc.tt("pool", pt_[:, qs:qs + 128], pt_[:, qs:qs + 128], trib[:], ALU.mult,
                                          [rpt, r_trib], [rpt])
                                return pt_, rpt

                            def d2(kt, res_):
                                pt_, rpt = res_
                                j = kt - 4 * qb
                                for qi in range(max(0, j), 4):
                                    Sc.mm(p_O[:, qi * 65:qi * 65 + 65], pt_[:, qi * 128:(qi + 1) * 128], v_a[:, kt, :],
                                          False, False, [rpt, r_va], [rpO], skip=True)

                            pend = []
                            for kt in range(4 * qb + 4):
                                pend.append((kt, d1(kt)))
                                if len(pend) > NBUF_D - 2:
                                    d2(*pend.pop(0))
                            while pend:
                                d2(*pend.pop(0))

                            Sc.op("dve", lambda e, p_O=p_O, r_i=r_i: e.reciprocal(out=r_i[:], in_=p_O[:, 64:260:65]),
                                  [rpO], [r_ri])
                            for qi in range(4):
                                Sc.ts("dve", o_b[:, qi, :], p_O[:, qi * 65:qi * 65 + 64], r_i[:, qi:qi + 1], None,
                                      ALU.mult, None, [rpO, r_ri], [r_ob])
                            Sc.dma("sp", o_all.ap()[c0:c0 + 512, h * 64:(h + 1) * 64].rearrange("(q p) d -> p q d", p=128),
                                   o_b[:], reads=[r_ob])
                if stop_after == "D%d" % l:
                    return nc

                with Scope() as sc:
                    identf, r_idf = sc.sb([128, 128])
                    identb, r_idb = sc.sb([128, 128], BF16)
                    Sc.dma("sp", identf[:], cd["identf"].ap(), writes=[r_idf])
                    Sc.cp("dve", identb[:], identf[:], [r_idf], [r_idb])
                    gpost, r_gpost = sc.sb([128, 1024])
                    Sc.dma("sp", gpost[:], bcast_rows(Wl["g_post"], 0, 128, 1024), writes=[r_gpost])
                    wo, r_wo = sc.sb([128, 8, 1024], BF16)
                    load_cast(sc, wo[:], r_wo, Wl["w_o"].ap().rearrange("(k p) n -> p k n", p=128), [128, 8, 1024])
                    ot = [sc.sb([128, 1024], BF16) for _ in range(2)]
                    xt = [sc.sb([128, 1024]) for _ in range(2)]
                    ptr = [sc.ps([128, 1024], BF16) for _ in range(2)]
                    oT = [sc.sb([128, 8, 128], BF16) for _ in range(2)]
                    py = [sc.ps() for _ in range(4)]
                    junk, r_junk = sc.sb([128, 1024], BF16)
                    ysb = [sc.sb([128, 1024]) for _ in range(2)]
                    xo = [sc.sb([128, 1024]) for _ in range(2)]
                    def front(ti):
                        o_t, r_ot = ot[ti % 2]
                        x_t, r_x = xt[ti % 2]
                        p_t, r_pt = ptr[ti % 2]
                        o_T, r_oT = oT[ti % 2]
                        rows = slice(ti * 128, (ti + 1) * 128)
                        Sc.dma("sp", o_t[:], o_all.ap()[rows, :], writes=[r_ot])
                        Sc.dma(dq(), x_t[:], x_src[rows, :], writes=[r_x])
                        for k in range(8):
                            Sc.tr(p_t[:, k * 128:(k + 1) * 128], o_t[:, k * 128:(k + 1) * 128], identb[:],
                                  [r_ot, r_idb], [r_pt])
                        Sc.cp("act", o_T[:], p_t[:].rearrange("p (k t) -> p k t", k=8), [r_pt], [r_oT])
                        for hf in range(2):
                            p_y, rpy = py[(ti % 2) * 2 + hf]
                            for k in range(8):
                                Sc.mm(p_y[:], o_T[:, k, :], wo[:, k, hf * 512:(hf + 1) * 512], k == 0, k == 7,
                                      [r_oT, r_wo], [rpy])

                    def back(ti):
                        x_t, r_x = xt[ti % 2]
                        y_s, r_ys = ysb[ti % 2]
                        x_o, r_xo = xo[ti % 2]
                        rows = slice(ti * 128, (ti + 1) * 128)
                        for hf in range(2):
                            p_y, rpy = py[(ti % 2) * 2 + hf]
                            Sc.cp("act" if hf else "dve", y_s[:, hf * 512:(hf + 1) * 512], p_y[:], [rpy], [r_ys])
                        ss, rss = rms_rstd(sc, y_s[:], 1024, junk[:], r_junk, [r_ys])
                        Sc.stt(y_s[:], y_s[:], ss[:, 0:1], gpost[:], ALU.mult, ALU.mult, [r_ys, rss, r_gpost], [r_ys])
                        Sc.tt("pool", x_o[:], y_s[:], x_t[:], ALU.add, [r_ys, r_x], [r_xo])
                        Sc.dma("sp", xs1.ap()[rows, :], x_o[:], reads=[r_xo])

                    front(0)
                    for ti in range(NT):
                        if ti + 1 < NT:
                            front(ti + 1)
                        back(ti)
                if stop_after == "E%d" % l:
                    return nc

                with Scope() as sc:
                    identf, r_idf = sc.sb([128, 128])
                    identb, r_idb = sc.sb([128, 128], BF16)
                    Sc.dma("sp", identf[:], cd["identf"].ap(), writes=[r_idf])
                    Sc.cp("dve", identb[:], identf[:], [r_idf], [r_idb])
                    gf, r_gf = sc.sb([128, 1024])
                    Sc.dma("sp", gf[:], bcast_rows(Wl["g_fpre"], 0, 128, 1024), writes=[r_gf])
                    wg, r_wg = sc.sb([128, 8, 2816], BF16)
                    wu, r_wu = sc.sb([128, 8, 2816], BF16)
                    stg = [sc.sb([128, 8, 352]) for _ in range(2)]
                    for wi, (wt_, rw_, src) in enumerate(((wg, r_wg, Wl["w_gate"]), (wu, r_wu, Wl["w_up"]))):
                        v = src.ap().rearrange("(k p) n -> p k n", p=128)
                        for cc in range(8):
                            load_cast(sc, wt_[:, :, cc * 352:(cc + 1) * 352], rw_, v[:, :, cc * 352:(cc + 1) * 352], None,
                                      stage=stg[(wi * 8 + cc) % 2])
                    cw, r_cw = sc.sb([128, 66])
                    cb, r_cb = sc.sb([128, 22])
                    Sc.dma("sp", cw[:], Wl["conv_w"].ap(), writes=[r_cw])
                    Sc.dma("sp", cb[:], Wl["conv_b"].ap(), writes=[r_cb])
                    gprev, r_gprev = sc.sb([128, 22, 2])
                    Sc.memset("dve", gprev[:], 0.0, [r_gprev])
                    xt = [sc.sb([128, 1024]) for _ in range(2)]
                    junk, r_junk = sc.sb([128, 1024], BF16)
                    hb = [sc.sb([128, 1024], BF16) for _ in range(2)]
                    ptr = [sc.ps([128, 1024], BF16) for _ in range(2)]
                    hT = [sc.sb([128, 8, 512], BF16) for _ in range(2)]
                    pg = [sc.ps() for _ in range(2)]
                    pu = [sc.ps() for _ in range(2)]
                    gsh = [sc.sb([128, 514]) for _ in range(2)]
                    gc = [sc.sb([128, 512]) for _ in range(2)]
                    ao = [sc.sb([128, 512], BF16) for _ in range(2)]
                    n = 0
                    for tb_ in range(NB):
                        c0 = tb_ * 512
                        hTt, r_hT = hT[tb_ % 2]
                        for i in range(4):
                            ti = tb_ * 4 + i
                            (x_t, r_x) = xt[ti % 2]
                            (h_b, r_hb) = hb[ti % 2]
                            (p_t, r_pt) = ptr[ti % 2]
                            Sc.dma(dq(), x_t[:], xs1.ap()[ti * 128:(ti + 1) * 128, :], writes=[r_x])
                            ss, rss = rms_rstd(sc, x_t[:], 1024, junk[:], r_junk, [r_x])
                            Sc.stt(h_b[:], x_t[:], ss[:, 0:1], gf[:], ALU.mult, ALU.mult, [r_x, rss, r_gf], [r_hb])
                            for k in range(8):
                                Sc.tr(p_t[:, k * 128:(k + 1) * 128], h_b[:, k * 128:(k + 1) * 128], identb[:],
                                      [r_hb, r_idb], [r_pt])
                            Sc.cp("act", hTt[:, :, i * 128:(i + 1) * 128],
                                  p_t[:].rearrange("p (k t) -> p k t", k=8), [r_pt], [r_hT])
                        for fc in range(22):
                            p_g, rpg = pg[n % 2]
                            p_u, rpu = pu[n % 2]
                            g_s, r_gs = gsh[n % 2]
                            g_c, r_gc = gc[n % 2]
                            a_o, r_ao = ao[n % 2]
                            n += 1
                            for k in range(8):
                                Sc.mm(p_g[:], wg[:, k, fc * 128:(fc + 1) * 128], hTt[:, k, :], k == 0, k == 7,
                                      [r_wg, r_hT], [rpg])
                            for k in range(8):
                                Sc.mm(p_u[:], wu[:, k, fc * 128:(fc + 1) * 128], hTt[:, k, :], k == 0, k == 7,
                                      [r_wu, r_hT], [rpu])
                            Sc.cp("pool", g_s[:, 0:2], gprev[:, fc, :], [r_gprev], [r_gs])
                            Sc.cp("act", g_s[:, 2:514], p_g[:], [rpg], [r_gs])
                            Sc.cp("pool", gprev[:, fc, :], g_s[:, 512:514], [r_gs], [r_gprev])
                            Sc.ts("dve", g_c[:], g_s[:, 2:514], cw[:, fc * 3 + 2:fc * 3 + 3], cb[:, fc:fc + 1], ALU.mult,
                                  ALU.add, [r_gs, r_cw, r_cb], [r_gc])
                            Sc.stt(g_c[:], g_s[:, 1:513], cw[:, fc * 3 + 1:fc * 3 + 2], g_c[:], ALU.mult, ALU.add,
                                   [r_gs, r_cw, r_gc], [r_gc])
                            Sc.stt(g_c[:], g_s[:, 0:512], cw[:, fc * 3:fc * 3 + 1], g_c[:], ALU.mult, ALU.add,
                                   [r_gs, r_cw, r_gc], [r_gc])
                            Sc.act(g_c[:], g_c[:], AF.Gelu_apprx_tanh, [r_gc], [r_gc])
                            Sc.tt("dve", a_o[:], g_c[:], p_u[:], ALU.mult, [r_gc, rpu], [r_ao])
                            Sc.dma(dq(), aT_d.ap()[fc * 128:(fc + 1) * 128, c0:c0 + 512], a_o[:], reads=[r_ao])
                if stop_after == "F1%d" % l:
                    return nc

                with Scope() as sc:
                    gp, r_gp = sc.sb([128, 1024])
                    Sc.dma("sp", gp[:], bcast_rows(Wl["g_fpost"], 0, 128, 1024), writes=[r_gp])
                    wd, r_wd = sc.sb([128, 22, 1024], BF16)
                    stg = [sc.sb([128, 2, 1024]) for _ in range(2)]
                    v = Wl["w_down"].ap().rearrange("(k p) n -> p k n", p=128)
                    for cc in range(11):
                        load_cast(sc, wd[:, cc * 2:cc * 2 + 2, :], r_wd, v[:, cc * 2:cc * 2 + 2, :], None, stage=stg[cc % 2])
                    aT = [sc.sb([128, 22, 128], BF16) for _ in range(2)]
                    xt = [sc.sb([128, 1024]) for _ in range(2)]
                    py = [sc.ps() for _ in range(4)]
                    junk, r_junk = sc.sb([128, 1024], BF16)
                    ysb = [sc.sb([128, 1024]) for _ in range(2)]
                    xo = [sc.sb([128, 1024]) for _ in range(2)]
                    def front(ti):
                        a_T, r_aT = aT[ti % 2]
                        x_t, r_x = xt[ti % 2]
                        rows = slice(ti * 128, (ti + 1) * 128)
                        dma_split("sp", a_T[:], aT_d.ap()[:, ti * 128:(ti + 1) * 128].rearrange("(k p) t -> p k t", p=128),
                                  1, 4, writes=[r_aT])
                        Sc.dma(dq(), x_t[:], xs1.ap()[rows, :], writes=[r_x])
                        for hf in range(2):
                            p_y, rpy = py[(ti % 2) * 2 + hf]
                            for k in range(22):
                                Sc.mm(p_y[:], a_T[:, k, :], wd[:, k, hf * 512:(hf + 1) * 512], k == 0, k == 21,
                                      [r_aT, r_wd], [rpy])

                    def back(ti):
                        x_t, r_x = xt[ti % 2]
                        y_s, r_ys = ysb[ti % 2]
                        x_o, r_xo = xo[ti % 2]
                        rows = slice(ti * 128, (ti + 1) * 128)
                        for hf in range(2):
                            p_y, rpy = py[(ti % 2) * 2 + hf]
                            Sc.cp("act" if hf else "dve", y_s[:, hf * 512:(hf + 1) * 512], p_y[:], [rpy], [r_ys])
                        ss, rss = rms_rstd(sc, y_s[:], 1024, junk[:], r_junk, [r_ys])
                        Sc.stt(y_s[:], y_s[:], ss[:, 0:1], gp[:], ALU.mult, ALU.mult, [r_ys, rss, r_gp], [r_ys])
                        Sc.tt("pool", x_o[:], y_s[:], x_t[:], ALU.add, [r_ys, r_x], [r_xo])
                        Sc.dma("sp", xs2.ap()[rows, :], x_o[:], reads=[r_xo])

                    front(0)
                    for ti in range(NT):
                        if ti + 1 < NT:
                            front(ti + 1)
                        back(ti)
                if stop_after == "F2%d" % l:
                    return nc

                with Scope() as sc:
                    identf, r_idf = sc.sb([128, 128])
                    identb, r_idb = sc.sb([128, 128], BF16)
                    Sc.dma("sp", identf[:], cd["identf"].ap(), writes=[r_idf])
                    Sc.cp("dve", identb[:], identf[:], [r_idf], [r_idb])
                    wpg, r_wpg = sc.sb([128, 8, 1024], BF16)
                    load_cast(sc, wpg[:], r_wpg, Wl["ple_gate"].ap().rearrange("(k p) n -> p k n", p=128), [128, 8, 1024])
                    wpp, r_wpp = sc.sb([128, 2, 1024], BF16)
                    load_cast(sc, wpp[:], r_wpp, Wl["ple_proj"].ap().rearrange("(k p) n -> p k n", p=128), [128, 2, 1024])
                    xt = [sc.sb([128, 1024]) for _ in range(2)]
                    pt_in = [sc.sb([128, 256]) for _ in range(2)]
                    xb = [sc.sb([128, 1280], BF16) for _ in range(2)]
                    ptr = [sc.ps([128, 1024], BF16) for _ in range(2)]
                    ptp = sc.ps([128, 1024], BF16)
                    xT = [sc.sb([128, 10, 128], BF16) for _ in range(2)]
                    pgt = [sc.ps() for _ in range(2)]
                    pe_ = [sc.ps() for _ in range(2)]
                    sg = [sc.sb([128, 1024]) for _ in range(2)]
                    xo = [sc.sb([128, 1024]) for _ in range(2)]
                    def front(ti):
                        x_t, r_x = xt[ti % 2]
                        p_i, r_pi = pt_in[ti % 2]
                        x_b, r_xb = xb[ti % 2]
                        p_t, r_pt = ptr[ti % 2]
                        x_T, r_xT = xT[ti % 2]
                        rows = slice(ti * 128, (ti + 1) * 128)
                        Sc.dma("sp", x_t[:], xs2.ap()[rows, :], writes=[r_x])
                        Sc.dma(dq(), p_i[:], p_in.ap()[l, bi_, rows, :], writes=[r_pi])
                        Sc.cp("dve", x_b[:, 0:1024], x_t[:], [r_x], [r_xb])
                        Sc.cp("pool", x_b[:, 1024:1280], p_i[:], [r_pi], [r_xb])
                        for k in range(8):
                            Sc.tr(p_t[:, k * 128:(k + 1) * 128], x_b[:, k * 128:(k + 1) * 128], identb[:],
                                  [r_xb, r_idb], [r_pt])
                        for k in range(2):
                            Sc.tr(ptp[0][:, k * 128:(k + 1) * 128], x_b[:, 1024 + k * 128:1024 + (k + 1) * 128], identb[:],
                                  [r_xb, r_idb], [ptp[1]])
                        Sc.cp("act", x_T[:, 0:8, :], p_t[:].rearrange("p (k t) -> p k t", k=8), [r_pt], [r_xT])
                        Sc.cp("act", x_T[:, 8:10, :], ptp[0][:, 0:256].rearrange("p (k t) -> p k t", k=2), [ptp[1]],
                              [r_xT])

                    def back(ti):
                        x_t, r_x = xt[ti % 2]
                        x_T, r_xT = xT[ti % 2]
                        s_g, r_sg = sg[ti % 2]
                        x_o, r_xo = xo[ti % 2]
                        rows = slice(ti * 128, (ti + 1) * 128)
                        for hf in range(2):
                            p_g, rpg = pgt[hf]
                            p_e, rpe = pe_[hf]
                            for k in range(8):
                                Sc.mm(p_g[:], x_T[:, k, :], wpg[:, k, hf * 512:(hf + 1) * 512], k == 0, k == 7,
                                      [r_xT, r_wpg], [rpg])
                            for k in range(2):
                                Sc.mm(p_e[:], x_T[:, 8 + k, :], wpp[:, k, hf * 512:(hf + 1) * 512], k == 0, k == 1,
                                      [r_xT, r_wpp], [rpe])
                            Sc.act(s_g[:, hf * 512:(hf + 1) * 512], p_g[:], AF.Sigmoid, [rpg], [r_sg])
                            Sc.tt("dve", s_g[:, hf * 512:(hf + 1) * 512], s_g[:, hf * 512:(hf + 1) * 512], p_e[:], ALU.mult,
                                  [r_sg, rpe], [r_sg])
                        Sc.tt("pool", x_o[:], s_g[:], x_t[:], ALU.add, [r_sg, r_x], [r_xo])
                        Sc.dma("sp", x_dst[rows, :], x_o[:], reads=[r_xo])

                    front(0)
                    for ti in range(NT):
                        if ti + 1 < NT:
                            front(ti + 1)
                        back(ti)
                x_src = xs3.ap()
    return nc


def prep_weights(l, w_in, mla_q_norm, mla_w_uq, mla_kv_norm, mla_w_ukv, nsa_cmp_pos, nsa_cmp_w1, nsa_cmp_w2,
                 w_o, ffn_w_gate, ffn_w_up, ffn_conv_w, ffn_conv_b, ffn_w_down, ple_proj, ple_gate,
                 attn_pre_norm, attn_post_norm, ffn_pre_norm, ffn_post_norm):
    f = lambda a: np.ascontiguousarray(np.asarray(a, dtype=np.float32))
    wi = np.asarray(w_in[l], dtype=np.float32)
    offs = np.cumsum([0, 256, 128, 32, 512, 128, 128, 128, 128, 128, 128, 24])
    seg = lambda i: wi[:, offs[i]:offs[i + 1]]
    c_q, c_kv, k_rope, q_nsa, k_cmp, v_cmp, k_slc, v_slc, k_win, v_win, g_nsa = [seg(i) for i in range(11)]
    d = {}
    d["w_fm"] = f(np.concatenate([q_nsa, k_cmp, v_cmp, k_slc, k_win], axis=1))
    d["w_kr"] = f(np.concatenate([k_rope, k_rope[:, 16:32], k_rope[:, 0:16]], axis=1))
    d["w_tm"] = f(np.concatenate([c_q, c_kv, v_slc, v_win, g_nsa], axis=1))
    uq = np.asarray(mla_w_uq[l], dtype=np.float32).reshape(256, 8, 96)
    d["wuq_n"] = f(uq[:, :, 0:64].reshape(256, 512))
    ra = uq[:, :, 64:96]
    rb = np.concatenate([uq[:, :, 80:96], uq[:, :, 64:80]], axis=2)
    d["wuq_r"] = f(np.concatenate([ra.reshape(256, 256), rb.reshape(256, 256)], axis=1))
    ukv = np.asarray(mla_w_ukv[l], dtype=np.float32).reshape(128, 8, 128)
    d["wukv_k"] = f(ukv[:, :, 0:64].reshape(128, 512))
    d["wukv_v"] = f(ukv[:, :, 64:128].reshape(128, 512))
    d["qn_g"] = f(np.asarray(mla_q_norm[l]).reshape(1, 256))
    d["kvn_g"] = f(np.asarray(mla_kv_norm[l]).reshape(1, 128))
    d["cmp_pos"] = f(np.asarray(nsa_cmp_pos[l]).transpose(0, 2, 1))
    w1 = np.asarray(nsa_cmp_w1[l], dtype=np.float32).reshape(2, 32, 64, 128)
    d["cmp_w1"] = f(w1.transpose(0, 2, 1, 3).reshape(2, 64, 32 * 128))
    d["cmp_w2"] = f(nsa_cmp_w2[l])
    d["w_o"] = f(w_o[l])
    d["w_gate"] = f(ffn_w_gate[l])
    d["w_up"] = f(ffn_w_up[l])
    cw = np.asarray(ffn_conv_w[l], dtype=np.float32)
    d["conv_w"] = f(cw.T.reshape(22, 128, 3).transpose(1, 0, 2).reshape(128, 66))
    d["conv_b"] = f(np.asarray(ffn_conv_b[l], dtype=np.float32).reshape(22, 128).T)
    d["w_down"] = f(ffn_w_down[l])
    d["ple_proj"] = f(ple_proj[l])
    d["ple_gate"] = f(ple_gate[l])
    d["g_pre"] = f(np.asarray(attn_pre_norm[l]).reshape(1, 1024))
    d["g_post"] = f(np.asarray(attn_post_norm[l]).reshape(1, 1024))
    d["g_fpre"] = f(np.asarray(ffn_pre_norm[l]).reshape(1, 1024))
    d["g_fpost"] = f(np.asarray(ffn_post_norm[l]).reshape(1, 1024))
    return d


def make_in_maps(x, p, positions, rel_bias, L, ncores, nbc=1, **w):
    B, S, _ = x.shape
    consts = make_consts(S)
    shared = {"relb": np.ascontiguousarray(np.asarray(rel_bias, dtype=np.float32))}
    for k, v in consts.items():
        shared["c_" + k] = np.ascontiguousarray(v, dtype=np.float32)
    for l in range(L):
        for k, v in prep_weights(l, **w).items():
            shared["l%d_%s" % (l, k)] = v
    maps = []
    for c in range(ncores):
        b0 = (c * nbc) % B
        m = dict(shared)
        m["x"] = np.ascontiguousarray(np.asarray(x[b0:b0 + nbc], dtype=np.float32))
        m["p"] = np.ascontiguousarray(np.asarray(p[:, b0:b0 + nbc], dtype=np.float32))
        m["pos"] = np.ascontiguousarray(np.asarray(positions[b0:b0 + nbc], dtype=np.int32).reshape(nbc, S))
        maps.append(m)
    return maps


N_CORES = 4


def kernel(x, p, positions, rel_bias, **w):
    x = np.asarray(x)
    B, S, D = x.shape
    L = np.asarray(p).shape[0]
    nbc = B // N_CORES
    nc = build(S=S, L=L, NBC=nbc)
    maps = make_in_maps(x, np.asarray(p), np.asarray(positions), rel_bias, L, N_CORES, nbc=nbc, **w)
    res = run_bass_kernel_spmd(nc, maps, core_ids=list(range(N_CORES)))
    out = np.concatenate([np.asarray(res.results[c]["out"], dtype=np.float32) for c in range(N_CORES)], axis=0)
    return out.reshape(B, S, D)
```
